# Optimizing a Trainium2 kernel written in Bass

```python
import math
import jax
import jax.numpy as jnp
from jax import lax
import numpy as np

D_MODEL = 2048
BATCH = 1
SEQ = 8192
DEPTH = 1

D_MIX = D_MODEL
M_HEADS = 4
M_QK_DIM = 128
M_V_DIM = D_MIX // 2 // M_HEADS
G_HEADS = 8
G_HEAD_DIM = D_MIX // 2 // G_HEADS
CONV_WIDTH = 5
CHUNK = 64
GATE_SOFTCAP = 15.0
N_EXPERTS = 64
TOP_K = 6
N_GROUPS = 8
TOPK_GROUPS = 4
D_EXPERT = 1408
D_SHARED = 1408
ROUTED_SCALE = 2.5
EXPERT_BLOCK = 256
NORM_EPS = 1e-6
DEEPNORM_ALPHA = (2.0 * DEPTH) ** 0.25
DEEPNORM_BETA = (8.0 * DEPTH) ** -0.25

M_QK = M_HEADS * M_QK_DIM
M_V = M_HEADS * M_V_DIM
G_W = G_HEADS * G_HEAD_DIM
IN_SPLIT = (M_QK, M_QK, M_V, M_V, 2 * M_HEADS, 2 * M_HEADS, 3 * G_W, G_W, 2 * G_HEADS, 2 * G_HEADS)
IN_COLS = sum(IN_SPLIT)

kernel_name = 'hybrid_mlstm_gdn_moe_deepnorm_adaln'


def layer_norm(x):
    x32 = x.astype(jnp.float32)
    mu = jnp.mean(x32, -1, keepdims=True)
    var = jnp.mean(jnp.square(x32 - mu), -1, keepdims=True)
    return ((x32 - mu) * lax.rsqrt(var + NORM_EPS)).astype(x.dtype)


def rms_norm(x):
    return x * lax.rsqrt(jnp.mean(jnp.square(x), -1, keepdims=True) + NORM_EPS)


def l2_norm(x):
    return x * lax.rsqrt(jnp.sum(jnp.square(x), -1, keepdims=True) + NORM_EPS)


def softcap(x):
    return GATE_SOFTCAP * jnp.tanh(x / GATE_SOFTCAP)


def split_cols(a, sizes):
    return jnp.split(a, np.cumsum(sizes)[:-1].tolist(), axis=-1)


def to_chunks(a):
    b, h, t = a.shape[:3]
    return jnp.moveaxis(a.reshape(b, h, t // CHUNK, CHUNK, *a.shape[3:]), 2, 0)


def from_chunks(a):
    nc, b, h, l = a.shape[:4]
    return jnp.moveaxis(a, 0, 2).reshape(b, h, nc * l, *a.shape[4:])


def centred_depthwise_conv(x, w):
    return lax.conv_general_dilated(
        x, w[:, None, :].astype(x.dtype), (1,), [(CONV_WIDTH // 2, CONV_WIDTH // 2)],
        dimension_numbers=('NWC', 'WIO', 'NWC'), feature_group_count=x.shape[-1])


def mlstm_chunkwise(q, k, v, log_i, log_f):
    b, h, _, dk = q.shape
    dv = v.shape[-1]
    tri = jnp.tril(jnp.ones((CHUNK, CHUNK), bool))

    def step(carry, inp):
        c_st, n_st, m_st = carry
        qc, kc, vc, ic, fc = inp
        bcum = jnp.cumsum(fc, axis=-1)
        d = jnp.where(tri, bcum[..., :, None] - bcum[..., None, :] + ic[..., None, :], -jnp.inf)
        inter = bcum + m_st[..., None]
        m_row = jnp.maximum(jnp.max(d, -1), inter)
        w = jnp.exp(d - m_row[..., None]) * jnp.einsum('bhid,bhjd->bhij', qc, kc)
        s_inter = jnp.exp(inter - m_row)
        num = jnp.einsum('bhij,bhje->bhie', w, vc) + s_inter[..., None] * jnp.einsum('bhid,bhde->bhie', qc, c_st)
        den = jnp.sum(w, -1) + s_inter * jnp.einsum('bhid,bhd->bhi', qc, n_st)
        out = num / jnp.maximum(jnp.abs(den), jnp.exp(-m_row))[..., None]
        b_last = bcum[..., -1]
        w_log = b_last[..., None] - bcum + ic
        m_new = jnp.maximum(b_last + m_st, jnp.max(w_log, -1))
        carry_decay = jnp.exp(b_last + m_st - m_new)
        w_add = jnp.exp(w_log - m_new[..., None])
        c_st = carry_decay[..., None, None] * c_st + jnp.einsum('bhj,bhjd,bhje->bhde', w_add, kc, vc)
        n_st = carry_decay[..., None] * n_st + jnp.einsum('bhj,bhjd->bhd', w_add, kc)
        return (c_st, n_st, m_new), out

    init = (jnp.zeros((b, h, dk, dv), jnp.float32), jnp.zeros((b, h, dk), jnp.float32),
            jnp.zeros((b, h), jnp.float32))
    _, out = lax.scan(step, init, (to_chunks(q), to_chunks(k), to_chunks(v), to_chunks(log_i), to_chunks(log_f)))
    return from_chunks(out)


def gated_delta_chunkwise(q, k, v, g, beta):
    qc, kc, vc, gc, bc = (to_chunks(a) for a in (q, k, v, g, beta))
    nc, b, h, l, dk = qc.shape
    dv = vc.shape[-1]
    tri = jnp.tril(jnp.ones((CHUNK, CHUNK), bool))
    strict = jnp.tril(jnp.ones((CHUNK, CHUNK), bool), -1)
    gcum = jnp.cumsum(gc, -1)
    decay = jnp.exp(jnp.where(tri, gcum[..., :, None] - gcum[..., None, :], -jnp.inf))
    kb = kc * bc[..., None]
    a_strict = jnp.where(strict, jnp.einsum('cbhid,cbhjd->cbhij', kb, kc) * decay, 0.0)
    rhs = jnp.concatenate([vc * bc[..., None], kb * jnp.exp(gcum)[..., None]], -1)
    sol = lax.linalg.triangular_solve(a_strict + jnp.eye(CHUNK, dtype=jnp.float32), rhs,
                                      left_side=True, lower=True, unit_diagonal=True)
    u, w = sol[..., :dv], sol[..., dv:]
    attn = jnp.einsum('cbhid,cbhjd->cbhij', qc, kc) * decay

    def step(s, inp):
        qj, kj, uj, wj, gj, aj = inp
        v_new = uj - jnp.einsum('bhid,bhde->bhie', wj, s)
        out = (jnp.einsum('bhid,bhde->bhie', qj * jnp.exp(gj)[..., None], s)
               + jnp.einsum('bhij,bhje->bhie', aj, v_new))
        g_last = gj[..., -1:]
        s = (s * jnp.exp(g_last)[..., None]
             + jnp.einsum('bhjd,bhje->bhde', kj * jnp.exp(g_last - gj)[..., None], v_new))
        return s, out

    _, out = lax.scan(step, jnp.zeros((b, h, dk, dv), jnp.float32), (qc, kc, u, w, gcum, attn))
    return from_chunks(out)


def bidirectional(scan_fn, q, k, v, gates):
    flip = lambda a: jnp.flip(a, axis=2)
    fwd = scan_fn(q, k, v, *(g[0] for g in gates))
    bwd = scan_fn(flip(q), flip(k), flip(v), *(flip(g[1]) for g in gates))
    return fwd + flip(bwd)


def hybrid_token_mixer(u, w_in, m_igate_bias, m_fgate_bias, m_norm_w, g_conv_w, g_A_log, g_dt_bias, g_norm_w, w_out):
    bsz, seq, _ = u.shape
    f32 = jnp.float32
    proj = u @ w_in
    mq, mk, mv, mo, mi, mf, gqkv, gz, gb, ga = split_cols(proj, IN_SPLIT)
    heads = lambda a, n, d: a.reshape(bsz, seq, n, d).transpose(0, 2, 1, 3).astype(f32)
    gates = lambda a, n: a.reshape(bsz, seq, 2, n).transpose(2, 0, 3, 1).astype(f32)
    q = heads(mq, M_HEADS, M_QK_DIM)
    k = heads(mk, M_HEADS, M_QK_DIM) * M_QK_DIM ** -0.5
    v = heads(mv, M_HEADS, M_V_DIM)
    log_i = softcap(gates(mi, M_HEADS) + m_igate_bias[:, None, :, None])
    log_f = jax.nn.log_sigmoid(softcap(gates(mf, M_HEADS) + m_fgate_bias[:, None, :, None]))
    hm = bidirectional(mlstm_chunkwise, q, k, v, (log_i, log_f))
    hm = rms_norm(hm).transpose(0, 2, 1, 3).reshape(bsz, seq, M_V) * m_norm_w * jax.nn.sigmoid(mo.astype(f32))
    qkv = jax.nn.silu(centred_depthwise_conv(gqkv, g_conv_w).astype(f32))
    gq, gk, gv = jnp.split(qkv, 3, axis=-1)
    q = l2_norm(heads(gq, G_HEADS, G_HEAD_DIM)) * G_HEAD_DIM ** -0.5
    k = l2_norm(heads(gk, G_HEADS, G_HEAD_DIM))
    v = heads(gv, G_HEADS, G_HEAD_DIM)
    beta = jax.nn.sigmoid(gates(gb, G_HEADS))
    log_decay = -jnp.exp(g_A_log.astype(f32))[:, None, :, None] * jax.nn.softplus(
        gates(ga, G_HEADS) + g_dt_bias[:, None, :, None])
    hg = bidirectional(gated_delta_chunkwise, q, k, v, (log_decay, beta))
    hg = (rms_norm(hg) * g_norm_w).transpose(0, 2, 1, 3).reshape(bsz, seq, G_W) * jax.nn.silu(gz.astype(f32))
    mixed = jnp.concatenate([hm, hg], -1).astype(u.dtype)
    return mixed @ w_out


def routed_experts(tok, top_idx, top_w, e_gate, e_up, e_down):
    n, d = tok.shape
    m = n * TOP_K
    n_blocks = (m + N_EXPERTS * (EXPERT_BLOCK - 1) + EXPERT_BLOCK - 1) // EXPERT_BLOCK
    flat_e = top_idx.reshape(-1)
    order = jnp.argsort(flat_e)
    e_sorted = flat_e[order]
    counts = jnp.bincount(flat_e, length=N_EXPERTS)
    padded = (counts + EXPERT_BLOCK - 1) // EXPERT_BLOCK * EXPERT_BLOCK
    start = jnp.cumsum(counts) - counts
    padded_end = jnp.cumsum(padded)
    dest = (padded_end - padded)[e_sorted] + jnp.arange(m) - start[e_sorted]
    n_slots = n_blocks * EXPERT_BLOCK
    slot_tok = jnp.zeros((n_slots,), jnp.int32).at[dest].set((order // TOP_K).astype(jnp.int32))
    slot_w = jnp.zeros((n_slots,), tok.dtype).at[dest].set(top_w.reshape(-1)[order].astype(tok.dtype))
    block_e = jnp.minimum(jnp.searchsorted(padded_end, jnp.arange(n_blocks) * EXPERT_BLOCK, side='right'),
                          N_EXPERTS - 1)

    def expert_block(args):
        idx, wts, e = args
        xb = tok[idx]
        hb = jax.nn.silu(xb @ e_gate[e]) * (xb @ e_up[e])
        return (hb @ e_down[e]) * wts[:, None]

    yb = lax.map(expert_block, (slot_tok.reshape(n_blocks, EXPERT_BLOCK),
                                slot_w.reshape(n_blocks, EXPERT_BLOCK), block_e))
    return jnp.zeros_like(tok).at[slot_tok].add(yb.reshape(-1, d))


def moe_ffn(u, router_w, router_bias, e_gate, e_up, e_down, s_gate, s_up, s_down):
    bsz, seq, d = u.shape
    f32 = jnp.float32
    tok = u.reshape(-1, d)
    n = tok.shape[0]
    scores = jax.nn.sigmoid((tok @ router_w).astype(f32))
    biased = scores + router_bias.astype(f32)
    group_score = jnp.sum(lax.top_k(biased.reshape(n, N_GROUPS, N_EXPERTS // N_GROUPS), 2)[0], -1)
    _, top_groups = lax.top_k(group_score, TOPK_GROUPS)
    group_mask = jnp.sum(jax.nn.one_hot(top_groups, N_GROUPS, dtype=f32), 1) > 0
    expert_mask = jnp.repeat(group_mask, N_EXPERTS // N_GROUPS, axis=1)
    _, top_idx = lax.top_k(jnp.where(expert_mask, biased, -jnp.inf), TOP_K)
    top_s = jnp.take_along_axis(scores, top_idx, axis=1)
    top_w = ROUTED_SCALE * top_s / jnp.sum(top_s, -1, keepdims=True)
    routed = routed_experts(tok, top_idx, top_w, e_gate, e_up, e_down)
    shared = (jax.nn.silu(tok @ s_gate) * (tok @ s_up)) @ s_down
    return (routed + shared).reshape(bsz, seq, d)


def setup_inputs(seed: int = 0) -> dict:
    key = jax.random.key(seed)
    ks = jax.random.split(key, 26)
    f32 = jnp.float32
    nrm = lambda k, shape, s: jax.random.normal(k, shape, f32) * s
    L, D, E = DEPTH, D_MODEL, N_EXPERTS
    beta = DEEPNORM_BETA
    col_scale = jnp.concatenate([
        jnp.ones((2 * M_QK,), f32), jnp.full((M_V,), beta, f32),
        jnp.ones((M_V + 4 * M_HEADS + 2 * G_W,), f32), jnp.full((G_W,), beta, f32),
        jnp.ones((G_W + 4 * G_HEADS,), f32)])
    dt = jnp.exp(jax.random.uniform(ks[10], (L, 2, G_HEADS), f32, math.log(1e-3), math.log(1e-1)))
    return {
        'x': nrm(ks[0], (BATCH, SEQ, D), 1.0),
        'c': nrm(ks[1], (BATCH, D), 1.0),
        'w_ada': nrm(ks[2], (L, D, 6 * D), 0.1 * D ** -0.5),
        'b_ada': nrm(ks[3], (L, 6 * D), 0.01),
        'w_in': nrm(ks[4], (L, D, IN_COLS), D ** -0.5) * col_scale,
        'm_igate_bias': nrm(ks[5], (L, 2, M_HEADS), 0.1),
        'm_fgate_bias': jnp.linspace(3.0, 6.0, M_HEADS, dtype=f32)[None, None, :] + nrm(ks[6], (L, 2, M_HEADS), 0.1),
        'm_norm_w': 1.0 + nrm(ks[7], (L, M_V), 0.02),
        'g_conv_w': nrm(ks[8], (L, CONV_WIDTH, 3 * G_W), CONV_WIDTH ** -0.5),
        'g_A_log': jnp.log(jax.random.uniform(ks[9], (L, 2, G_HEADS), f32, 1.0, 16.0)),
        'g_dt_bias': dt + jnp.log(-jnp.expm1(-dt)),
        'g_norm_w': 1.0 + nrm(ks[11], (L, G_HEAD_DIM), 0.02),
        'w_out': nrm(ks[12], (L, D_MIX, D), beta * D_MIX ** -0.5),
        'ln1_w': 1.0 + nrm(ks[13], (L, D), 0.02),
        'ln1_b': nrm(ks[14], (L, D), 0.02),
        'router_w': nrm(ks[15], (L, D, E), D ** -0.5),
        'router_bias': nrm(ks[16], (L, E), 0.01),
        'e_gate': nrm(ks[17], (L, E, D, D_EXPERT), D ** -0.5),
        'e_up': nrm(ks[18], (L, E, D, D_EXPERT), beta * D ** -0.5),
        'e_down': nrm(ks[19], (L, E, D_EXPERT, D), beta * D_EXPERT ** -0.5),
        's_gate': nrm(ks[20], (L, D, D_SHARED), D ** -0.5),
        's_up': nrm(ks[21], (L, D, D_SHARED), beta * D ** -0.5),
        's_down': nrm(ks[22], (L, D_SHARED, D), beta * D_SHARED ** -0.5),
        'ln2_w': 1.0 + nrm(ks[23], (L, D), 0.02),
        'ln2_b': nrm(ks[24], (L, D), 0.02),
    }


def reference(x, c, w_ada, b_ada, w_in, m_igate_bias, m_fgate_bias, m_norm_w, g_conv_w, g_A_log, g_dt_bias,
              g_norm_w, w_out, ln1_w, ln1_b, router_w, router_bias, e_gate, e_up, e_down, s_gate, s_up, s_down,
              ln2_w, ln2_b):
    h = x
    cond = jax.nn.silu(c)
    for layer in range(DEPTH):
        mod = (cond @ w_ada[layer] + b_ada[layer])[:, None, :]
        shift1, scale1, gate1, shift2, scale2, gate2 = jnp.split(mod, 6, axis=-1)
        u = layer_norm(h) * (1 + scale1) + shift1
        y = hybrid_token_mixer(u, w_in[layer], m_igate_bias[layer], m_fgate_bias[layer], m_norm_w[layer],
                               g_conv_w[layer], g_A_log[layer], g_dt_bias[layer], g_norm_w[layer], w_out[layer])
        h = layer_norm(DEEPNORM_ALPHA * h + (1 + gate1) * y) * ln1_w[layer] + ln1_b[layer]
        u = layer_norm(h) * (1 + scale2) + shift2
        y = moe_ffn(u, router_w[layer], router_bias[layer], e_gate[layer], e_up[layer], e_down[layer],
                    s_gate[layer], s_up[layer], s_down[layer])
        h = layer_norm(DEEPNORM_ALPHA * h + (1 + gate2) * y) * ln2_w[layer] + ln2_b[layer]
    return h
```

```python
import os
import contextlib
import numpy as np
import ml_dtypes
import concourse.bass as bass
import concourse.mybir as mybir
from concourse.bass_utils import run_bass_kernel_spmd

F32 = mybir.dt.float32
BF16 = mybir.dt.bfloat16
AF = mybir.ActivationFunctionType
ALU = mybir.AluOpType
AX = mybir.AxisListType

NCORES = 8
T = 8192
D = 2048
TS = T // NCORES
NTL = TS // 128
NCH = T // 128
E_LOC = int(os.environ.get('MK_ELOC', '8'))
DE = 1408
DSH = 176
NORM_EPS = 1e-6
ALPHA = 2.0 ** 0.25
BIG = 30000.0
NW = 1032
C_GQ, C_GK, C_GV, C_MQ, C_MK = 0, 128, 256, 384, 512
C_TM = 640
C_MV, C_MO, C_GZ, C_GATE = 0, 128, 256, 384
NTM = 392


class Sched:
    def __init__(self, nc, n_dma_sems=10):
        self.nc = nc
        self.eng = {"pe": nc.tensor, "act": nc.scalar, "dve": nc.vector, "pool": nc.gpsimd, "sp": nc.sync}
        self.sem, self.cnt, self._ctx = {}, {}, []
        for e in ("pe", "act", "dve", "pool"):
            c = nc.semaphore("sem_" + e)
            self.sem[e] = c.__enter__()
            self._ctx.append(c)
            self.cnt[e] = 0
        self.ring = {}
        for q in ("sp", "pool", "act"):
            lst = []
            for i in range(n_dma_sems):
                c = nc.semaphore("dq_%s_%d" % (q, i))
                lst.append([c.__enter__(), 0])
                self._ctx.append(c)
            self.ring[q] = [lst, 0]
        c = nc.semaphore("sem_cc")
        self.cc_sem = c.__enter__()
        self._ctx.append(c)
        self.cc_cnt = 0
        self.last_w, self.readers = {}, {}
        self.waited = {e: {} for e in self.eng}
        self.n_ops = 0

    def close(self):
        for c in reversed(self._ctx):
            c.__exit__(None, None, None)

    def _wait(self, e, tok):
        s, v = tok
        w = self.waited[e]
        if w.get(id(s), 0) >= v:
            return
        w[id(s)] = v
        self.eng[e].wait_ge(s, v)

    def _deps(self, e, reads, writes):
        toks = []
        for k in list(reads) + list(writes):
            t = self.last_w.get(k)
            if t is not None:
                toks.append(t)
        for k in writes:
            toks.extend(self.readers.get(k, ()))
        own = self.sem.get(e)
        for t in toks:
            if e == "pe" and t[0] is own:
                continue
            self._wait(e, t)

    def _commit(self, tok, reads, writes):
        for k in writes:
            self.last_w[k] = tok
            self.readers[k] = []
        for k in reads:
            lst = self.readers.setdefault(k, [])
            lst.append(tok)
            if len(lst) > 16:
                best = {}
                for t in lst:
                    if id(t[0]) not in best or best[id(t[0])][1] < t[1]:
                        best[id(t[0])] = t
                self.readers[k] = list(best.values())

    def op(self, e, reads, writes, fn):
        self._deps(e, reads, writes)
        ins = fn()
        self.cnt[e] += 1
        ins.then_inc(self.sem[e], 1)
        tok = (self.sem[e], self.cnt[e])
        self._commit(tok, reads, writes)
        self.n_ops += 1
        return tok

    def dma(self, q, out, in_, reads, writes, **kw):
        lst, pos = self.ring[q]
        slot = lst[pos]
        self.ring[q][1] = (pos + 1) % len(lst)
        if slot[1] > 0:
            self._wait(q, (slot[0], slot[1]))
        self._deps(q, reads, writes)
        ins = self.eng[q].dma_start(out=out, in_=in_, **kw)
        ins.then_inc(slot[0], 16)
        slot[1] += 16
        tok = (slot[0], slot[1])
        self._commit(tok, reads, writes)
        self.n_ops += 1
        return tok

    def coll(self, kind, op, in_ap, out_ap, reads, writes):
        self._deps("pool", reads, writes)
        ins = self.nc.gpsimd.collective_compute(kind, op, replica_groups=[list(range(NCORES))],
                                                ins=[in_ap], outs=[out_ap])
        self.cc_cnt += 1
        ins.then_inc(self.cc_sem, 1)
        tok = (self.cc_sem, self.cc_cnt)
        self._commit(tok, reads, writes)
        return tok

    def barrier(self):
        toks = [(self.sem[o], self.cnt[o]) for o in self.sem if self.cnt[o] > 0]
        for q in self.ring:
            for slot in self.ring[q][0]:
                if slot[1] > 0:
                    toks.append((slot[0], slot[1]))
        if self.cc_cnt:
            toks.append((self.cc_sem, self.cc_cnt))
        for e in self.eng:
            for t in toks:
                if e in self.sem and t[0] is self.sem[e]:
                    continue
                self._wait(e, t)
        self.last_w, self.readers = {}, {}

    def wait_keys(self, e, keys):
        for k in keys:
            t = self.last_w.get(k)
            if t is not None:
                self._wait(e, t)


def build_program(stage=99, dbg=False):
    nc = bass.Bass("TRN2", target_bir_lowering=False)
    S = Sched(nc)
    es = contextlib.ExitStack()

    def din(name, shape, dt=F32):
        return nc.dram_tensor(name, list(shape), dt, kind="ExternalInput").ap()

    def dout(name, shape, dt=F32):
        return nc.dram_tensor(name, list(shape), dt, kind="ExternalOutput").ap()

    def dscr(name, shape, dt=F32):
        return nc.dram_tensor(name, list(shape), dt, kind="Internal").ap()

    def sb(name, shape, dt=F32, stack=None):
        return (stack or es).enter_context(nc.sbuf_tensor("sb_" + name, list(shape), dt))

    V, A, P, PE = nc.vector, nc.scalar, nc.gpsimd, nc.tensor

    x_in = din("x", [TS, D])
    c_in = din("c", [D])
    wada_in = din("w_ada", [D, 1536])
    bada_in = din("b_ada", [1, 1536])
    win_in = din("w_in", [D, NW])
    gbias_in = din("gate_bias", [16])
    mnw_in = din("m_norm_w", [128])
    selh_in = din("sel_head", [8])
    esel_in = din("esel", [9, 128, 128])
    gnw_in = din("g_norm_w", [128])
    cw_in = din("conv_w", [3, 128, 5])
    wout_in = din("w_out", [256, D])
    ln1w_in, ln1b_in = din("ln1_w", [D]), din("ln1_b", [D])
    ln2w_in, ln2b_in = din("ln2_w", [D]), din("ln2_b", [D])
    rw_in = din("router_w", [D, 64])
    rb_in = din("router_bias", [64])
    ident_in = din("ident", [128, 128])
    masks_in = din("masks", [6, 128, 128])
    mneg_in = din("mneg", [2, 4, 128, 512])
    if stage >= 7:
        eg_in = din("e_gate", [E_LOC, D, DE])
        eu_in = din("e_up", [E_LOC, D, DE])
        ed_in = din("e_down", [E_LOC, DE, D])
        sg_in = din("s_gate", [D, 256])
        su_in = din("s_up", [D, 256])
        sd_in = din("s_down", [256, D])
    out_ap = dout("out", [TS, D])
    dbgs = {}

    ag0_in, ag0_out = dscr("ag0_in", [1, 1536]), dscr("ag0_out", [8, 1536])
    ag1_in, ag1_out = dscr("ag1_in", [D, TS], BF16), dscr("ag1_out", [NCORES * D, TS], BF16)
    pf = dscr("pf", [5, 128, T + 4])
    pt = dscr("pt", [T, NTM])

    ident = sb("ident", [128, 128])
    identb = sb("identb", [128, 128], BF16)
    ones = sb("ones", [128, 128])
    S.dma("sp", ident[:], ident_in[:, :], [], ["ident"])
    S.op("dve", ["ident"], ["identb"], lambda: V.tensor_copy(out=identb[:], in_=ident[:]))
    S.op("dve", [], ["ones"], lambda: V.memset(ones[:], 1.0))

    psb = [es.enter_context(nc.psum_tensor("ps%d" % i, [128, 512], F32)) for i in range(7)]
    pstb = es.enter_context(nc.psum_tensor("pstb", [128, 1024], BF16))

    modT = sb("modT", [128, 96])
    with contextlib.ExitStack() as st:
        ct = sb("ct", [128, 16], stack=st)
        wt = sb("wada_t", [128, 16, 512], stack=st)
        brow = sb("brow", [1, 1536], stack=st)
        mrow = sb("mrow", [1, 1536], stack=st)
        S.dma("sp", ct[:], c_in.rearrange("(k p) -> p k", p=128), [], ["ct"], allow_slow_non_contiguous=True)
        S.dma("sp", brow[:], bada_in[:, :], [], ["brow"])
        S.op("act", ["ct"], ["ct"], lambda: A.activation(out=ct[:], in_=ct[:], func=AF.Silu))
        for g in range(3):
            S.dma("sp", wt[:], wada_in[:, g * 512:(g + 1) * 512].rearrange("(k p) n -> p k n", p=128), [], ["wt"])

            def mm():
                for k in range(16):
                    ins = PE.matmul(psb[0][0:1, :], lhsT=ct[:, k:k + 1], rhs=wt[:, k, :], start=(k == 0), stop=(k == 15))
                return ins
            S.op("pe", ["ct", "wt"], ["ps0"], mm)
            S.op("dve", ["ps0", "brow"], ["mrow"],
                 lambda: V.tensor_tensor(out=mrow[:, g * 512:(g + 1) * 512], in0=psb[0][0:1, :],
                                         in1=brow[:, g * 512:(g + 1) * 512], op=ALU.add))
        S.dma("sp", ag0_in[:, :], mrow[:], ["mrow"], ["ag0_in"])
        S.coll("AllGather", ALU.bypass, ag0_in[:, :], ag0_out[:, :], ["ag0_in"], ["ag0_out"])
        S.dma("sp", modT[:], ag0_out.rearrange("a (b p) -> p (a b)", p=128), ["ag0_out"], ["modT"],
              allow_slow_non_contiguous=True)
        S.barrier()
    mod_flat = ag0_out.rearrange("a b -> (a b)")
    S.op("dve", ["modT"], ["modT"], lambda: V.tensor_scalar_add(out=modT[:, 16:32], in0=modT[:, 16:32], scalar1=1.0))
    S.op("dve", ["modT"], ["modT"], lambda: V.tensor_scalar_add(out=modT[:, 64:80], in0=modT[:, 64:80], scalar1=1.0))

    def layer_norm_tile(stk, src, dst_bf, key_src, key_dst, tag):
        stt = sb("ln_st" + tag, [128, 4, 6], stack=stk)
        mv = sb("ln_mv" + tag, [128, 2], stack=stk)
        rstd = sb("ln_rs" + tag, [128, 1], stack=stk)

        def run(src_ap, dst_ap, ks, kd):
            for i in range(4):
                S.op("dve", [ks], ["lnst" + tag], lambda: V.bn_stats(out=stt[:, i, :], in_=src_ap[:, i * 512:(i + 1) * 512]))
            S.op("dve", ["lnst" + tag], ["lnmv" + tag], lambda: V.bn_aggr(out=mv[:], in_=stt[:]))
            S.op("dve", ["lnmv" + tag], ["lnrs" + tag], lambda: V.tensor_scalar_add(out=rstd[:], in0=mv[:, 1:2], scalar1=NORM_EPS))
            S.op("act", ["lnrs" + tag], ["lnrs" + tag], lambda: A.sqrt(out=rstd[:], in_=rstd[:]))
            S.op("dve", ["lnrs" + tag], ["lnrs" + tag], lambda: V.reciprocal(out=rstd[:], in_=rstd[:]))
            S.op("dve", [ks, "lnmv" + tag, "lnrs" + tag], [kd],
                 lambda: V.tensor_scalar(out=dst_ap, in0=src_ap, scalar1=mv[:, 0:1], scalar2=rstd[:, 0:1],
                                         op0=ALU.subtract, op1=ALU.mult))
        return run

    with contextlib.ExitStack() as st:
        xt = [sb("xt%d" % i, [128, D], stack=st) for i in range(2)]
        xn = [sb("xn%d" % i, [128, D], BF16, stack=st) for i in range(2)]
        uall = sb("u1T_all", [128, 16, TS], BF16, stack=st)
        ln = layer_norm_tile(st, None, None, None, None, "a")
        for i in range(NTL):
            b = i % 2
            S.dma("sp", xt[b][:], x_in[i * 128:(i + 1) * 128, :], [], ["xt%d" % b])
            ln(xt[b][:], xn[b][:], "xt%d" % b, "xn%d" % b)
            for h in range(2):
                def tr():
                    for j in range(8):
                        ins = PE.transpose(out=pstb[:, j * 128:(j + 1) * 128], in_=xn[b][:, (h * 8 + j) * 128:(h * 8 + j + 1) * 128],
                                           identity=identb[:])
                    return ins
                S.op("pe", ["xn%d" % b, "identb"], ["pstb"], tr)
                for j in range(8):
                    jj = h * 8 + j
                    S.op("act", ["pstb", "modT"], ["uall"],
                         lambda: A.activation(out=uall[:, jj, i * 128:(i + 1) * 128], in_=pstb[:, j * 128:(j + 1) * 128],
                                              func=AF.Identity, scale=modT[:, 16 + jj:17 + jj], bias=modT[:, jj:jj + 1]))
        S.dma("sp", ag1_in.rearrange("(j p) t -> p j t", p=128), uall[:], ["uall"], ["ag1_in"])
        S.coll("AllGather", ALU.bypass, ag1_in[:, :], ag1_out[:, :], ["ag1_in"], ["ag1_out"])
        S.barrier()
    if stage == 1:
        d1 = dout("dbg_u1T", [D, TS], BF16)
        dm = dout("dbg_mod", [128, 96])
        S.dma("sp", d1[:, :], ag1_out[2 * D:3 * D, :], [], ["d1"])
        S.dma("sp", dm[:, :], modT[:], ["modT"], ["dm"])
        S.barrier()
        S.close(); es.close()
        return nc

    with contextlib.ExitStack() as st:
        W = sb("W_in", [128, 16, NW], BF16, stack=st)
        for k4 in range(4):
            S.dma("pool", W[:, k4 * 4:(k4 + 1) * 4, :],
                  win_in[k4 * 512:(k4 + 1) * 512, :].rearrange("(k p) n -> p k n", p=128), [], ["W%d" % k4])
        Wk = ["W0", "W1", "W2", "W3"]
        U = [sb("U%d" % i, [128, 16, TS], BF16, stack=st) for i in range(2)]
        fst = [sb("fst%d" % i, [128, 5, 512], stack=st) for i in range(2)]
        tst = [sb("tst%d" % i, [128, NTM], stack=st) for i in range(2)]
        zpad = sb("zpad", [128, 5, 2], stack=st)
        S.op("dve", [], ["zpad"], lambda: V.memset(zpad[:], 0.0))
        S.dma("sp", pf[:, :, 0:2].rearrange("s p t -> p s t"), zpad[:], ["zpad"], ["pf"], allow_slow_non_contiguous=True)
        S.dma("sp", pf[:, :, T + 2:T + 4].rearrange("s p t -> p s t"), zpad[:], ["zpad"], ["pf"], allow_slow_non_contiguous=True)
        cntf = 0
        cntt = 0
        for r in range(NCORES):
            ub = r % 2
            S.dma("sp", U[ub][:], ag1_out[r * D:(r + 1) * D, :].rearrange("(j p) t -> p j t", p=128), [], ["U%d" % ub])
            for tb in range(2):
                fb = cntf % 2
                cntf += 1
                for m in range(5):
                    bank = psb[m % 2]

                    def mm():
                        for k in range(16):
                            ins = PE.matmul(bank[:, :], lhsT=W[:, k, m * 128:(m + 1) * 128], rhs=U[ub][:, k, tb * 512:(tb + 1) * 512],
                                            start=(k == 0), stop=(k == 15))
                        return ins
                    S.op("pe", ["U%d" % ub] + Wk, ["ps%d" % (m % 2)], mm)
                    if m % 2 == 0:
                        S.op("act", ["ps%d" % (m % 2)], ["fst%d" % fb], lambda: A.copy(out=fst[fb][:, m, :], in_=bank[:, :]))
                    else:
                        S.op("dve", ["ps%d" % (m % 2)], ["fst%d" % fb], lambda: V.tensor_copy(out=fst[fb][:, m, :], in_=bank[:, :]))
                t0 = r * TS + tb * 512
                S.dma("sp", pf[:, :, 2 + t0:2 + t0 + 512].rearrange("s p t -> p s t"), fst[fb][:], ["fst%d" % fb], ["pf"])
            for i in range(NTL):
                tbuf = cntt % 2
                cntt += 1

                def mm2():
                    for k in range(16):
                        ins = PE.matmul(psb[2][:, 0:NTM], lhsT=U[ub][:, k, i * 128:(i + 1) * 128], rhs=W[:, k, C_TM:NW],
                                        start=(k == 0), stop=(k == 15))
                    return ins
                S.op("pe", ["U%d" % ub] + Wk, ["ps2"], mm2)
                if i % 2 == 0:
                    S.op("act", ["ps2"], ["tst%d" % tbuf], lambda: A.copy(out=tst[tbuf][:, :], in_=psb[2][:, 0:NTM]))
                else:
                    S.op("dve", ["ps2"], ["tst%d" % tbuf], lambda: V.tensor_copy(out=tst[tbuf][:, :], in_=psb[2][:, 0:NTM]))
                t0 = r * TS + i * 128
                S.dma("sp", pt[t0:t0 + 128, :], tst[tbuf][:], ["tst%d" % tbuf], ["pt"])
        S.barrier()
    if stage == 2:
        d1 = dout("dbg_pf", [5, 128, 1028])
        d1b = dout("dbg_pfb", [5, 128, 1028])
        d2 = dout("dbg_pt", [1024, NTM])
        S.dma("sp", d1[:, :, :], pf[:, :, 0:1028], [], ["d1"])
        S.dma("sp", d1b[:, :, :], pf[:, :, T - 1024:T + 4], [], ["d1b"])
        S.dma("sp", d2[:, :], pt[5 * 1024 + 512:6 * 1024 + 512, :], [], ["d2"])
        S.barrier()
        S.close(); es.close()
        return nc

    gT = dscr("gT", [3, 128, T])
    gtok = dscr("gtok", [2, T, 128])
    ag2_in = dscr("ag2_in", [3 * T, 128])
    es3 = contextlib.ExitStack()
    msk = sb("msk", [128, 6, 128], stack=es3)
    S.dma("sp", msk[:], masks_in.rearrange("m p i -> p m i"), [], ["msk"])
    GS = sb("GS", [128, 28, 64], stack=es3)
    mss = sb("mss", [128, NCH], stack=es3)
    QI = {n: i for i, n in enumerate(
        ["li_f", "li_b", "lf_f", "lf_b", "Ff", "Rb", "bias_f", "bias_b", "lnb_f", "lnb_b", "beta_f", "beta_b",
         "g_f", "g_b", "gam_f", "gam_b", "gL_f", "gL_b", "ngam_f", "ngam_b", "glb_f", "glb_b", "beg_f", "beg_b",
         "eLg_f", "eLg_b", "tmp0", "tmp1"])}

    def gs(name):
        return GS[:, QI[name], :]

    with contextlib.ExitStack() as st:
        cw = sb("cw", [128, 3, 5], stack=st)
        S.dma("sp", cw[:], cw_in.rearrange("s p w -> p s w"), [], ["cw"])
        cin = [sb("cin%d" % i, [128, 516], stack=st) for i in range(2)]
        acc = [sb("cacc%d" % i, [128, 512], stack=st) for i in range(2)]
        gout = [sb("gout%d" % i, [128, 512], stack=st) for i in range(2)]
        tko = [sb("tko%d" % i, [128, 512], stack=st) for i in range(2)]
        sq = sb("csq", [128, 512], stack=st)
        rn = sb("crn", [128, 512], stack=st)
        cnt = 0
        for b in range(T // 512):
            for s_ in range(3):
                i2 = cnt % 2
                cnt += 1
                kc, ka, kg, kt = "cin%d" % i2, "cacc%d" % i2, "gout%d" % i2, "tko%d" % i2
                S.dma("sp", cin[i2][:], pf[s_, :, 512 * b:512 * b + 516], [], [kc])
                S.op("dve", [kc, "cw"], [ka], lambda: V.tensor_scalar_mul(out=acc[i2][:], in0=cin[i2][:, 0:512], scalar1=cw[:, s_, 0:1]))
                for w in range(1, 5):
                    S.op("dve", [kc, "cw", ka], [ka],
                         lambda: V.scalar_tensor_tensor(out=acc[i2][:], in0=cin[i2][:, w:w + 512], scalar=cw[:, s_, w:w + 1],
                                                        in1=acc[i2][:], op0=ALU.mult, op1=ALU.add))
                S.op("act", [ka], [ka], lambda: A.activation(out=acc[i2][:], in_=acc[i2][:], func=AF.Silu))
                if s_ < 2:
                    S.op("dve", [ka], ["csq"], lambda: V.tensor_tensor(out=sq[:], in0=acc[i2][:], in1=acc[i2][:], op=ALU.mult))
                    S.op("pe", ["csq", "ones"], ["ps0"], lambda: PE.matmul(psb[0][:, :], lhsT=ones[:], rhs=sq[:], start=True, stop=True))
                    S.op("dve", ["ps0"], ["crn"], lambda: V.tensor_scalar_add(out=rn[:], in0=psb[0][:, :], scalar1=NORM_EPS))
                    S.op("act", ["crn"], ["crn"], lambda: A.sqrt(out=rn[:], in_=rn[:]))
                    S.op("dve", ["crn"], ["crn"], lambda: V.reciprocal(out=rn[:], in_=rn[:]))
                    sc = 128.0 ** -0.5 if s_ == 0 else 1.0
                    S.op("dve", ["crn", ka], [kg],
                         lambda: V.scalar_tensor_tensor(out=gout[i2][:], in0=rn[:], scalar=sc, in1=acc[i2][:], op0=ALU.mult, op1=ALU.mult))
                    src, ks = gout[i2], kg
                else:
                    src, ks = acc[i2], ka
                S.dma("sp", gT[s_, :, 512 * b:512 * b + 512], src[:], [ks], ["gT"])
                if s_ >= 1:
                    def tr():
                        for u in range(4):
                            ins = PE.transpose(out=psb[1][:, u * 128:(u + 1) * 128], in_=src[:, u * 128:(u + 1) * 128], identity=ident[:])
                        return ins
                    S.op("pe", [ks, "ident"], ["ps1"], tr)
                    S.op("act", ["ps1"], [kt], lambda: A.copy(out=tko[i2][:], in_=psb[1][:, :]))
                    S.dma("sp", gtok[s_ - 1, 512 * b:512 * b + 512, :].rearrange("(u p) d -> p u d", p=128),
                          tko[i2][:].rearrange("p (u d) -> p u d", u=4), [kt], ["gtok"])
        S.barrier()

    if stage == 2.5:
        dgt = dout("dbg_gT", [3, 128, 1024])
        dgk = dout("dbg_gtok", [2, 1024, 128])
        S.dma("sp", dgt[:, :, :], gT[:, :, 3072:4096], [], ["dgt"])
        S.dma("sp", dgk[:, :, :], gtok[:, 3072:4096, :], [], ["dgk"])
        S.barrier()
        S.close(); es.close()
        return nc
    with contextlib.ExitStack() as st:
        graw = sb("graw", [128, 64, 8], stack=st)
        gb = sb("gb", [128, 16], stack=st)
        al = sb("al", [128, 2], stack=st)
        tmp = [sb("gtmp%d" % i, [128, 64], stack=st) for i in range(3)]
        tot128 = sb("tot128", [128, 128], stack=st)
        S.op("dve", [], ["gtmp0"], lambda: V.memset(tot128[:], 0.0))
        S.dma("sp", graw[:], pt[:, C_GATE:C_GATE + 8].rearrange("(n p) g -> p n g", p=128), [], ["graw"],
              allow_slow_non_contiguous=True)
        S.dma("sp", gb[:], gbias_in.partition_broadcast(128), [], ["gb"])
        S.op("act", ["gb"], ["al"], lambda: A.activation(out=al[:], in_=gb[:, 8:10], func=AF.Exp))
        S.op("dve", ["al"], ["al"], lambda: V.tensor_scalar_mul(out=al[:], in0=al[:], scalar1=-1.0))
        K_ = "GS"

        def dv(fn, extra=()):
            S.op("dve", [K_] + list(extra), [K_], fn)

        def ac(fn, extra=()):
            S.op("act", [K_] + list(extra), [K_], fn)
        for d, sfx in enumerate(("_f", "_b")):
            xi = gs("tmp0")
            dv(lambda: V.tensor_scalar_add(out=xi, in0=graw[:, :, 0 + d], scalar1=gb[:, 0 + d:1 + d]), ["graw", "gb"])
            ac(lambda: A.activation(out=xi, in_=xi, func=AF.Tanh, scale=1.0 / 15.0))
            dv(lambda: V.tensor_scalar_mul(out=gs("li" + sfx), in0=xi, scalar1=15.0))
            dv(lambda: V.tensor_scalar_add(out=xi, in0=graw[:, :, 2 + d], scalar1=gb[:, 2 + d:3 + d]), ["graw", "gb"])
            ac(lambda: A.activation(out=xi, in_=xi, func=AF.Tanh, scale=1.0 / 15.0))
            ac(lambda: A.activation(out=xi, in_=xi, func=AF.Exp, scale=-15.0))
            dv(lambda: V.tensor_scalar_add(out=xi, in0=xi, scalar1=1.0))
            ac(lambda: A.activation(out=xi, in_=xi, func=AF.Ln))
            dv(lambda: V.tensor_scalar_mul(out=gs("lf" + sfx), in0=xi, scalar1=-1.0))
            ac(lambda: A.activation(out=xi, in_=graw[:, :, 4 + d], func=AF.Exp, scale=-1.0), ["graw"])
            dv(lambda: V.tensor_scalar_add(out=xi, in0=xi, scalar1=1.0))
            ac(lambda: A.activation(out=xi, in_=xi, func=AF.Ln))
            dv(lambda: V.tensor_scalar_mul(out=gs("lnb" + sfx), in0=xi, scalar1=-1.0))
            ac(lambda: A.activation(out=gs("beta" + sfx), in_=gs("lnb" + sfx), func=AF.Exp))
            z = gs("tmp1")
            dv(lambda: V.tensor_scalar_add(out=z, in0=graw[:, :, 6 + d], scalar1=gb[:, 6 + d:7 + d]), ["graw", "gb"])
            ac(lambda: A.activation(out=xi, in_=z, func=AF.Abs))
            ac(lambda: A.activation(out=xi, in_=xi, func=AF.Exp, scale=-1.0))
            dv(lambda: V.tensor_scalar_add(out=xi, in0=xi, scalar1=1.0))
            ac(lambda: A.activation(out=xi, in_=xi, func=AF.Ln))
            dv(lambda: V.tensor_scalar_max(out=z, in0=z, scalar1=0.0))
            dv(lambda: V.tensor_tensor(out=z, in0=z, in1=xi, op=ALU.add))
            dv(lambda: V.tensor_scalar_mul(out=gs("g" + sfx), in0=z, scalar1=al[:, d:d + 1]), ["al"])
            tri = msk[:, d, :]

            def mmc():
                PE.matmul(psb[2][:, 0:64], lhsT=tri, rhs=gs("g" + sfx), start=True, stop=True)
                PE.matmul(psb[2][:, 64:128], lhsT=ones[:], rhs=gs("g" + sfx), start=True, stop=True)
                PE.matmul(psb[2][:, 128:192], lhsT=tri, rhs=gs("lf" + sfx), start=True, stop=True)
                return PE.matmul(psb[2][:, 192:256], lhsT=ones[:], rhs=gs("lf" + sfx), start=True, stop=True)
            S.op("pe", [K_, "msk", "ones"], ["ps2"], mmc)
            S.op("dve", ["ps2"], [K_], lambda: V.tensor_copy(out=gs("gam" + sfx), in_=psb[2][:, 0:64]))
            S.op("act", ["ps2"], [K_], lambda: A.activation(out=gs("gL" + sfx), in_=psb[2][:, 64:128], func=AF.Exp))
            S.op("dve", ["ps2", K_], [K_], lambda: V.tensor_tensor(out=xi, in0=psb[2][:, 64:128], in1=gs("gam" + sfx), op=ALU.subtract))
            ac(lambda: A.activation(out=gs("eLg" + sfx), in_=xi, func=AF.Exp))
            dv(lambda: V.tensor_scalar_mul(out=gs("ngam" + sfx), in0=gs("gam" + sfx), scalar1=-1.0))
            dv(lambda: V.tensor_tensor(out=gs("glb" + sfx), in0=gs("gam" + sfx), in1=gs("lnb" + sfx), op=ALU.add))
            ac(lambda: A.activation(out=gs("beg" + sfx), in_=gs("glb" + sfx), func=AF.Exp))
            S.op("dve", ["ps2"], ["gtmp0"], lambda: V.tensor_copy(out=tot128[:, 0:64], in_=psb[2][:, 192:256]))
            S.op("pe", ["gtmp0", "ident"], ["ps3"], lambda: PE.transpose(out=psb[3][:, 0:128], in_=tot128[:], identity=ident[:]))
            tT = sb("totT%d" % d, [128, 128], stack=st)
            S.op("act", ["ps3"], ["totT"], lambda: A.copy(out=tT[:], in_=psb[3][:, 0:128]))
            S.op("pe", ["totT", "msk"], ["ps3"], lambda: PE.matmul(psb[3][:, 128:192], lhsT=tT[:], rhs=msk[:, 2 + d, 0:64], start=True, stop=True))
            FN = "Ff" if d == 0 else "Rb"
            S.op("dve", ["ps3", "ps2"], ["gtmp2"], lambda: V.tensor_copy(out=tmp[2][:], in_=psb[3][:, 128:192]))
            S.op("dve", ["ps2", "gtmp2"], [K_], lambda: V.tensor_tensor(out=gs(FN), in0=psb[2][:, 128:192], in1=tmp[2][:], op=ALU.add))
            dv(lambda: V.tensor_tensor(out=xi, in0=gs("li" + sfx), in1=gs(FN), op=ALU.subtract))
            dv(lambda: V.tensor_scalar_add(out=gs("bias" + sfx), in0=xi, scalar1=float(np.log(128.0 ** -0.5))))
        S.barrier()
    if stage == 3:
        dg = dout("dbg_GS", [128, 28, 64])
        dgt = dout("dbg_gT", [3, 128, 1024])
        dgk = dout("dbg_gtok", [2, 1024, 128])
        S.dma("sp", dg[:, :, :], GS[:], [], ["dg"])
        S.dma("sp", dgt[:, :, :], gT[:, :, 3072:4096], [], ["dgt"])
        S.dma("sp", dgk[:, :, :], gtok[:, 3072:4096, :], [], ["dgk"])
        S.barrier()
        S.close(); es.close()
        return nc

    with contextlib.ExitStack() as st:
        kTb = sb("kTb", [128, T], BF16, stack=st)
        qTb = sb("qTb", [128, T], BF16, stack=st)
        vaug = sb("vaug", [128, NCH, 129], BF16, stack=st)
        mng = sb("mng", [128, 2, 4, 512], stack=st)
        for hh in range(4):
            S.dma("pool", kTb[:, hh * 2048:(hh + 1) * 2048], pf[4, :, 2 + hh * 2048:2 + (hh + 1) * 2048], [], ["kTb"])
            S.dma("pool", qTb[:, hh * 2048:(hh + 1) * 2048], pf[3, :, 2 + hh * 2048:2 + (hh + 1) * 2048], [], ["qTb"])
        for hh in range(8):
            S.dma("pool", vaug[:, hh * 8:(hh + 1) * 8, 0:128],
                  pt[hh * 1024:(hh + 1) * 1024, C_MV:C_MV + 128].rearrange("(n p) e -> p n e", p=128), [], ["vaug"])
        S.op("dve", [], ["vaug1"], lambda: V.memset(vaug[:, :, 128:129], 1.0))
        S.dma("sp", mng[:], mneg_in.rearrange("d r p t -> p d r t"), [], ["mng"])
        dgm = sb("dgm", [128, 4, 128], stack=st)
        Fb = sb("Fb", [128, 512], stack=st)
        arg = sb("marg", [128, 512], stack=st)
        Dt = [sb("Dt%d" % i, [128, 512], stack=st) for i in range(2)]
        Pt = [sb("Pt%d" % i, [128, 512], BF16, stack=st) for i in range(2)]
        den = sb("mden", [128, 4], stack=st)
        msq = sb("msq", [128, 4, 128], stack=st)
        hacc = [sb("hacc%d" % i, [128, 4, 128], stack=st) for i in range(2)]
        mot = [sb("mot%d" % i, [128, 4, 128], stack=st) for i in range(2)]
        for b in range(T // 512):
            hb = b % 2
            kh = "hacc%d" % hb
            for d in range(2):
                Fcol = gs("Ff") if d == 0 else gs("Rb")
                bias = gs("bias_f") if d == 0 else gs("bias_b")
                for u in range(4):
                    S.op("dve", ["GS", "ident"], ["dgm"],
                         lambda: V.tensor_scalar_mul(out=dgm[:, u, :], in0=ident[:], scalar1=Fcol[:, 4 * b + u:4 * b + u + 1]))

                def mmF():
                    for u in range(4):
                        ins = PE.matmul(psb[4][:, u * 128:(u + 1) * 128], lhsT=ones[:], rhs=dgm[:, u, :], start=True, stop=True)
                    return ins
                S.op("pe", ["dgm", "ones"], ["ps4"], mmF)
                S.op("act", ["ps4"], ["Fb"], lambda: A.copy(out=Fb[:], in_=psb[4][:, :]))
                tiles = list(range(0, 4 * b + 4)) if d == 0 else list(range(4 * b, NCH))
                def qk(idx_):
                    a_ = tiles[idx_]
                    sb_ = idx_ % 2
                    S.op("pe", ["kTb", "qTb"], ["ps%d" % sb_],
                         lambda: PE.matmul(psb[sb_][:, :], lhsT=kTb[:, a_ * 128:(a_ + 1) * 128], rhs=qTb[:, b * 512:(b + 1) * 512],
                                           start=True, stop=True))
                qk(0)
                for idx, a in enumerate(tiles):
                    sbk = idx % 2
                    if idx + 1 < len(tiles):
                        qk(idx + 1)
                    if 4 * b <= a < 4 * b + 4:
                        S.op("dve", ["Fb", "mng"], ["marg"],
                             lambda: V.tensor_tensor(out=arg[:], in0=Fb[:], in1=mng[:, d, a - 4 * b, :], op=ALU.add))
                        src, ksrc = arg, "marg"
                    else:
                        src, ksrc = Fb, "Fb"
                    S.op("act", [ksrc, "GS"], ["Dt%d" % sbk],
                         lambda: A.activation(out=Dt[sbk][:], in_=src[:], func=AF.Exp, bias=bias[:, a:a + 1], scale=1.0))
                    S.op("dve", ["ps%d" % sbk, "Dt%d" % sbk], ["Pt%d" % sbk],
                         lambda: V.tensor_tensor(out=Pt[sbk][:], in0=psb[sbk][:, :], in1=Dt[sbk][:], op=ALU.mult))

                    def pv():
                        for u in range(4):
                            bank = psb[(2, 3, 5, 6)[u]]
                            col = 0
                            ins = PE.matmul(bank[:, col:col + 129], lhsT=Pt[sbk][:, u * 128:(u + 1) * 128], rhs=vaug[:, a, :],
                                            start=(idx == 0), stop=(idx == len(tiles) - 1))
                        return ins
                    S.op("pe", ["Pt%d" % sbk, "vaug", "vaug1"], ["ps2", "ps3", "ps5", "ps6"], pv)
                for u in range(4):
                    bank = psb[(2, 3, 5, 6)[u]]
                    col = 0
                    S.op("act", ["ps2", "ps3", "ps5", "ps6"], ["mden"], lambda: A.activation(out=den[:, u:u + 1], in_=bank[:, col + 128:col + 129], func=AF.Abs))
                S.op("dve", ["mden"], ["mden"], lambda: V.tensor_scalar_max(out=den[:], in0=den[:], scalar1=1.0))
                S.op("dve", ["mden"], ["mden"], lambda: V.reciprocal(out=den[:], in_=den[:]))
                for u in range(4):
                    bank = psb[(2, 3, 5, 6)[u]]
                    col = 0
                    if d == 0:
                        S.op("dve", ["ps2", "ps3", "ps5", "ps6", "mden"], [kh],
                             lambda: V.tensor_scalar_mul(out=hacc[hb][:, u, :], in0=bank[:, col:col + 128], scalar1=den[:, u:u + 1]))
                    else:
                        S.op("dve", ["ps2", "ps3", "ps5", "ps6", "mden", kh], [kh],
                             lambda: V.scalar_tensor_tensor(out=hacc[hb][:, u, :], in0=bank[:, col:col + 128], scalar=den[:, u:u + 1],
                                                            in1=hacc[hb][:, u, :], op0=ALU.mult, op1=ALU.add))
            S.op("dve", [kh], ["msq"], lambda: V.tensor_tensor(out=msq[:], in0=hacc[hb][:], in1=hacc[hb][:], op=ALU.mult))
            S.op("dve", ["msq"], ["mss"], lambda: V.tensor_reduce(out=mss[:, 4 * b:4 * b + 4], in_=msq[:], axis=AX.X, op=ALU.add))
            S.dma("sp", ag2_in[b * 512:(b + 1) * 512, :].rearrange("(u p) e -> p u e", p=128), hacc[hb][:], [kh], ["ag2_in"])
            km = "mot%d" % hb
            S.dma("sp", mot[hb][:], pt[b * 512:(b + 1) * 512, C_MO:C_MO + 128].rearrange("(u p) e -> p u e", p=128), [], [km])
            S.op("act", [km], [km], lambda: A.activation(out=mot[hb][:], in_=mot[hb][:], func=AF.Sigmoid))
            S.dma("sp", ag2_in[T + b * 512:T + (b + 1) * 512, :].rearrange("(u p) e -> p u e", p=128), mot[hb][:], [km], ["ag2_in"])
        S.barrier()

    with contextlib.ExitStack() as st:
        NM = sb("NM", [128, 4, 128], stack=st)
        S.op("dve", ["msk"], ["NM"], lambda: V.tensor_scalar(out=NM[:, 0, :], in0=msk[:, 0, :], scalar1=-1.0, scalar2=BIG, op0=ALU.add, op1=ALU.mult))
        S.op("dve", ["msk"], ["NM"], lambda: V.tensor_scalar(out=NM[:, 1, :], in0=msk[:, 1, :], scalar1=-1.0, scalar2=BIG, op0=ALU.add, op1=ALU.mult))
        for d in range(2):
            S.op("dve", ["NM", "ident"], ["NM"],
                 lambda: V.scalar_tensor_tensor(out=NM[:, 2 + d, :], in0=ident[:], scalar=-BIG, in1=NM[:, d, :], op0=ALU.mult, op1=ALU.add))
        Sst = [sb("Sst%d" % d, [128, 128], stack=st) for d in range(2)]
        for d in range(2):
            S.op("dve", [], ["Sst%d" % d], lambda: V.memset(Sst[d][:], 0.0))
        oacc = sb("oacc", [128, NCH, 128], stack=st)

        def dtile(name, shape=(128, 128), dt=F32):
            return [[sb("%s_%d_%d" % (name, d, p), list(shape), dt, stack=st) for p in range(2)] for d in range(2)]
        fm = dtile("fm", (128, 2, 128))
        tm = dtile("tm", (128, 2, 128))
        dg2 = dtile("dg2", (128, 2, 128))
        a1, a2, a3 = dtile("a1"), dtile("a2"), dtile("a3")
        ET, E2, E3, eG = dtile("ET"), dtile("E2"), dtile("E3"), dtile("eG")
        attnT, qeT = dtile("attnT"), dtile("qeT")
        XP = [[[sb("XP_%d_%d_%d" % (d, p, l), [128, 2, 128], stack=st) for l in range(2)] for p in range(1)] for d in range(2)]
        Xb = [[[sb("Xb_%d_%d_%d" % (d, p, l), [128, 128], stack=st) for l in range(2)] for p in range(1)] for d in range(2)]
        TT = dtile("TT")
        rv, rk, kd = dtile("rv"), dtile("rk"), dtile("kd")
        usb, wTsb, vnew = dtile("usb"), dtile("wTsb"), dtile("vnew")
        for step in range(NCH):
            par = step % 2
            for d in range(2):
                sfx = "_f" if d == 0 else "_b"
                n = step if d == 0 else NCH - 1 - step
                K = lambda nm: "%s_%d_%d" % (nm, d, par)
                c0, c1 = d * 256, d * 256 + 128
                pk = lambda bnk: "ps%d_%d" % (bnk, d)
                F, Tm = fm[d][par], tm[d][par]
                S.dma("sp", F[:], gT[0:2, :, n * 128:(n + 1) * 128].rearrange("s p t -> p s t"), [], [K("fm")])
                S.dma("sp", Tm[:], gtok[:, n * 128:(n + 1) * 128, :].rearrange("s p e -> p s e"), [], [K("tm")])
                qT_c, kT_c, k_tok, v_tok = F[:, 0, :], F[:, 1, :], Tm[:, 0, :], Tm[:, 1, :]
                gam, ngam, glb = gs("gam" + sfx), gs("ngam" + sfx), gs("glb" + sfx)

                def mm1():
                    PE.matmul(psb[0][:, c0:c0 + 128], lhsT=kT_c, rhs=kT_c, start=True, stop=True)
                    return PE.matmul(psb[0][:, c1:c1 + 128], lhsT=kT_c, rhs=qT_c, start=True, stop=True)
                S.op("pe", [K("fm")], [pk(0)], mm1)
                D2 = dg2[d][par]
                S.op("dve", ["GS", "ident"], [K("dg2")], lambda: V.tensor_scalar_mul(out=D2[:, 0, :], in0=ident[:], scalar1=gam[:, n:n + 1]))
                S.op("dve", ["GS", "ident"], [K("dg2")], lambda: V.tensor_scalar_mul(out=D2[:, 1, :], in0=ident[:], scalar1=glb[:, n:n + 1]))
                S.op("pe", [K("dg2"), "ones"], [pk(1)],
                     lambda: PE.matmul(psb[1][:, c0:c0 + 256], lhsT=ones[:], rhs=D2[:].rearrange("p a b -> p (a b)"), start=True, stop=True))
                G_ps, QK_ps = psb[0][:, c0:c0 + 128], psb[0][:, c1:c1 + 128]
                Gm_ps, GB_ps = psb[1][:, c0:c0 + 128], psb[1][:, c1:c1 + 128]
                A1, A2, A3 = a1[d][par], a2[d][par], a3[d][par]
                S.op("dve", [pk(1), "NM"], [K("a1")], lambda: V.tensor_tensor(out=A1[:], in0=Gm_ps, in1=NM[:, d, :], op=ALU.add))
                S.op("act", [K("a1"), "GS"], [K("ET")], lambda: A.activation(out=ET[d][par][:], in_=A1[:], func=AF.Exp, bias=ngam[:, n:n + 1], scale=1.0))
                S.op("dve", [pk(1), "NM"], [K("a2")], lambda: V.tensor_tensor(out=A2[:], in0=GB_ps, in1=NM[:, 2 + d, :], op=ALU.add))
                S.op("act", [K("a2"), "GS"], [K("E2")], lambda: A.activation(out=E2[d][par][:], in_=A2[:], func=AF.Exp, bias=ngam[:, n:n + 1], scale=1.0))
                S.op("dve", [pk(1), "NM"], [K("a3")], lambda: V.tensor_tensor(out=A3[:], in0=Gm_ps, in1=NM[:, 3 - d, :], op=ALU.subtract))
                S.op("act", [K("a3"), "GS"], [K("E3")], lambda: A.activation(out=E3[d][par][:], in_=A3[:], func=AF.Exp, bias=glb[:, n:n + 1], scale=-1.0))
                S.op("act", [pk(1)], [K("eG")], lambda: A.activation(out=eG[d][par][:], in_=Gm_ps, func=AF.Exp))
                xp0, xb0 = XP[d][0][0], Xb[d][0][0]
                S.op("dve", [pk(0), K("E2")], ["XP_%d_0" % d],
                     lambda: V.scalar_tensor_tensor(out=xp0[:, 0, :], in0=G_ps, scalar=-1.0, in1=E2[d][par][:], op0=ALU.mult, op1=ALU.mult))
                S.op("dve", [pk(0), K("E3")], ["Xb_%d_0" % d],
                     lambda: V.scalar_tensor_tensor(out=xb0[:], in0=G_ps, scalar=-1.0, in1=E3[d][par][:], op0=ALU.mult, op1=ALU.mult))
                S.op("dve", [pk(0), K("ET")], [K("attnT")], lambda: V.tensor_tensor(out=attnT[d][par][:], in0=QK_ps, in1=ET[d][par][:], op=ALU.mult))
                S.op("dve", [K("fm"), K("eG")], [K("qeT")], lambda: V.tensor_tensor(out=qeT[d][par][:], in0=qT_c, in1=eG[d][par][:], op=ALU.mult))
                S.op("dve", ["XP_%d_0" % d, "ident"], ["XP_%d_0" % d], lambda: V.tensor_tensor(out=xp0[:, 1, :], in0=xp0[:, 0, :], in1=ident[:], op=ALU.add))
                for l in range(7):
                    cur, nxt = l % 2, 1 - l % 2
                    xpc, xbc, xpn, xbn = XP[d][0][cur], Xb[d][0][cur], XP[d][0][nxt], Xb[d][0][nxt]
                    kxpc, kxbc, kxpn, kxbn = "XP_%d_%d" % (d, cur), "Xb_%d_%d" % (d, cur), "XP_%d_%d" % (d, nxt), "Xb_%d_%d" % (d, nxt)
                    if l < 6:
                        if l == 0:
                            S.op("pe", [kxpc, kxbc], [pk(2)], lambda: PE.matmul(psb[2][:, c0:c0 + 128], lhsT=xbc[:], rhs=xpc[:, 0, :], start=True, stop=True))
                        else:
                            S.op("pe", [kxpc, kxbc], [pk(2)],
                                 lambda: PE.matmul(psb[2][:, c0:c0 + 256], lhsT=xbc[:], rhs=xpc[:].rearrange("p a b -> p (a b)"), start=True, stop=True))
                        S.op("pe", [kxpc, kxbc], [pk(3)], lambda: PE.matmul(psb[3][:, c0:c0 + 128], lhsT=xpc[:, 0, :], rhs=xbc[:], start=True, stop=True))
                        S.op("act", [pk(2)], [kxpn], lambda: A.copy(out=xpn[:, 0, :], in_=psb[2][:, c0:c0 + 128]))
                        if l == 0:
                            S.op("dve", [kxpc], [kxpn], lambda: V.tensor_copy(out=xpn[:, 1, :], in_=xpc[:, 1, :]))
                        else:
                            S.op("dve", [kxpc, pk(2)], [kxpn], lambda: V.tensor_tensor(out=xpn[:, 1, :], in0=psb[2][:, c1:c1 + 128], in1=xpc[:, 1, :], op=ALU.add))
                        S.op("act", [pk(3)], [kxbn], lambda: A.copy(out=xbn[:], in_=psb[3][:, c0:c0 + 128]))
                    else:
                        S.op("pe", [kxpc, kxbc], [pk(2)], lambda: PE.matmul(psb[2][:, c0:c0 + 128], lhsT=xbc[:], rhs=xpc[:, 1, :], start=True, stop=True))
                        S.op("dve", [kxpc, pk(2)], [K("TT")], lambda: V.tensor_tensor(out=TT[d][par][:], in0=psb[2][:, c0:c0 + 128], in1=xpc[:, 1, :], op=ALU.add))
                RV, RK, KD = rv[d][par], rk[d][par], kd[d][par]
                S.op("dve", [K("tm"), "GS"], [K("rv")], lambda: V.tensor_scalar_mul(out=RV[:], in0=v_tok, scalar1=gs("beta" + sfx)[:, n:n + 1]))
                S.op("dve", [K("tm"), "GS"], [K("rk")], lambda: V.tensor_scalar_mul(out=RK[:], in0=k_tok, scalar1=gs("beg" + sfx)[:, n:n + 1]))
                S.op("dve", [K("tm"), "GS"], [K("kd")], lambda: V.tensor_scalar_mul(out=KD[:], in0=k_tok, scalar1=gs("eLg" + sfx)[:, n:n + 1]))

                def mm4():
                    PE.matmul(psb[4][:, c0:c0 + 128], lhsT=TT[d][par][:], rhs=RV[:], start=True, stop=True)
                    return PE.matmul(psb[4][:, c1:c1 + 128], lhsT=RK[:], rhs=TT[d][par][:], start=True, stop=True)
                S.op("pe", [K("TT"), K("rv"), K("rk")], [pk(4)], mm4)
                S.op("act", [pk(4)], [K("usb")], lambda: A.copy(out=usb[d][par][:], in_=psb[4][:, c0:c0 + 128]))
                S.op("act", [pk(4)], [K("wTsb")], lambda: A.copy(out=wTsb[d][par][:], in_=psb[4][:, c1:c1 + 128]))
                ks = "Sst%d" % d
                S.op("pe", [K("wTsb"), ks], [pk(5)], lambda: PE.matmul(psb[5][:, c0:c0 + 128], lhsT=wTsb[d][par][:], rhs=Sst[d][:], start=True, stop=True))
                S.op("dve", [pk(5), K("usb")], [K("vnew")], lambda: V.tensor_tensor(out=vnew[d][par][:], in0=usb[d][par][:], in1=psb[5][:, c0:c0 + 128], op=ALU.subtract))

                def mm5():
                    PE.matmul(psb[5][:, c1:c1 + 128], lhsT=qeT[d][par][:], rhs=Sst[d][:], start=True, stop=False)
                    PE.matmul(psb[5][:, c1:c1 + 128], lhsT=attnT[d][par][:], rhs=vnew[d][par][:], start=False, stop=True)
                    return PE.matmul(psb[6][:, c0:c0 + 128], lhsT=KD[:], rhs=vnew[d][par][:], start=True, stop=True)
                S.op("pe", [K("qeT"), K("attnT"), K("vnew"), K("kd"), ks], [pk(5) + "o", pk(6)], mm5)
                S.op("dve", [pk(6), ks, "GS"], [ks],
                     lambda: V.scalar_tensor_tensor(out=Sst[d][:], in0=Sst[d][:], scalar=gs("gL" + sfx)[:, n:n + 1], in1=psb[6][:, c0:c0 + 128],
                                                    op0=ALU.mult, op1=ALU.add))
                if step < NCH // 2:
                    S.op("act", [pk(5) + "o"], ["oacc%d" % n], lambda: A.copy(out=oacc[:, n, :], in_=psb[5][:, c1:c1 + 128]))
                else:
                    S.op("dve", [pk(5) + "o", "oacc%d" % n], ["oacc%d" % n],
                         lambda: V.tensor_tensor(out=oacc[:, n, :], in0=oacc[:, n, :], in1=psb[5][:, c1:c1 + 128], op=ALU.add))
        S.barrier()
        gwb = sb("gwb", [128, 128], stack=st)
        S.dma("sp", gwb[:], gnw_in.partition_broadcast(128), [], ["gwb"])
        ss = sb("gss", [128, NCH], stack=st)
        sq8 = sb("sq8", [128, 8, 128], stack=st)
        gz = [sb("gz%d" % i, [128, 8, 128], stack=st) for i in range(2)]
        for nb in range(NCH // 8):
            sl = slice(nb * 8, nb * 8 + 8)
            S.op("dve", ["oacc"], ["sq8"], lambda: V.tensor_tensor(out=sq8[:], in0=oacc[:, sl, :], in1=oacc[:, sl, :], op=ALU.mult))
            S.op("dve", ["sq8"], ["gss"], lambda: V.tensor_reduce(out=ss[:, sl], in_=sq8[:], axis=AX.X, op=ALU.add))
        S.op("dve", ["gss"], ["gss"], lambda: V.tensor_scalar(out=ss[:], in0=ss[:], scalar1=1.0 / 128.0, scalar2=NORM_EPS, op0=ALU.mult, op1=ALU.add))
        S.op("act", ["gss"], ["gss"], lambda: A.sqrt(out=ss[:], in_=ss[:]))
        S.op("dve", ["gss"], ["gss"], lambda: V.reciprocal(out=ss[:], in_=ss[:]))
        for nb in range(NCH // 8):
            zb = nb % 2
            kz = "gz%d" % zb
            S.dma("sp", gz[zb][:], pt[nb * 1024:(nb + 1) * 1024, C_GZ:C_GZ + 128].rearrange("(n p) e -> p n e", p=128), [], [kz])
            S.op("act", [kz], [kz], lambda: A.activation(out=gz[zb][:], in_=gz[zb][:], func=AF.Silu))
            for i in range(8):
                n = nb * 8 + i
                S.op("dve", ["oacc", "gss", "gwb"], ["oacc"],
                     lambda: V.scalar_tensor_tensor(out=oacc[:, n, :], in0=oacc[:, n, :], scalar=ss[:, n:n + 1], in1=gwb[:], op0=ALU.mult, op1=ALU.mult))
                S.op("dve", ["oacc", kz], [kz], lambda: V.tensor_tensor(out=gz[zb][:, i, :], in0=gz[zb][:, i, :], in1=oacc[:, n, :], op=ALU.mult))
            S.dma("sp", ag2_in[2 * T + nb * 1024:2 * T + (nb + 1) * 1024, :].rearrange("(n p) e -> p n e", p=128), gz[zb][:], [kz], ["ag2_in"])
        S.barrier()
    if stage == 4:
        dm = dout("dbg_mix", [3 * T, 128])
        S.dma("sp", dm[:, :], ag2_in[:, :], [], ["dm"])
        S.barrier()
        S.close(); es3.close(); es.close()
        return nc

    ssag_in, ssag_out = dscr("ssag_in", [128, NCH]), dscr("ssag_out", [NCORES * 128, NCH])
    rs1_in, rs1_out = dscr("rs1_in", [T, D]), dscr("rs1_out", [TS, D])
    with contextlib.ExitStack() as st:
        S.dma("sp", ssag_in[:, :], mss[:], ["mss"], ["ssag_in"])
        S.coll("AllGather", ALU.bypass, ssag_in[:, :], ssag_out[:, :], ["ssag_in"], ["ssag_out"])
        ssall = sb("ssall", [128, NCORES, NCH], stack=st)
        selb = sb("selb", [128, 8], stack=st)
        rst = sb("rst", [128, NCH], stack=st)
        S.dma("sp", ssall[:], ssag_out.rearrange("(c p) n -> p c n", p=128), ["ssag_out"], ["ssall"])
        S.dma("sp", selb[:], selh_in.partition_broadcast(128), [], ["selb"])
        S.op("dve", ["ssall", "selb"], ["rst"], lambda: V.tensor_scalar_mul(out=rst[:], in0=ssall[:, 0, :], scalar1=selb[:, 0:1]))
        for c_ in range(1, NCORES):
            S.op("dve", ["ssall", "selb", "rst"], ["rst"],
                 lambda: V.scalar_tensor_tensor(out=rst[:], in0=ssall[:, c_, :], scalar=selb[:, c_:c_ + 1], in1=rst[:], op0=ALU.mult, op1=ALU.add))
        S.op("dve", ["rst"], ["rst"], lambda: V.tensor_scalar(out=rst[:], in0=rst[:], scalar1=1.0 / 256.0, scalar2=NORM_EPS, op0=ALU.mult, op1=ALU.add))
        S.op("act", ["rst"], ["rst"], lambda: A.sqrt(out=rst[:], in_=rst[:]))
        S.op("dve", ["rst"], ["rst"], lambda: V.reciprocal(out=rst[:], in_=rst[:]))
        wo = sb("wo", [128, 2, D], BF16, stack=st)
        S.dma("pool", wo[:], wout_in.rearrange("(k p) n -> p k n", p=128), [], ["wo"])
        mnwb = sb("mnwb", [128, 128], stack=st)
        S.dma("sp", mnwb[:], mnw_in.partition_broadcast(128), [], ["mnwb"])
        m8 = [[sb("m8_%d_%d" % (a_, i), [128, 8, 128], stack=st) for i in range(2)] for a_ in range(3)]
        t1 = sb("mx_t1", [128, 128], stack=st)
        mixb = [sb("mixb%d" % i, [128, 2, 128], BF16, stack=st) for i in range(2)]
        mixT = [sb("mixT%d" % i, [128, 2, 128], BF16, stack=st) for i in range(2)]
        yst = [sb("yst%d" % i, [128, D], stack=st) for i in range(2)]
        for nb in range(NCH // 8):
            pb = nb % 2
            for a_ in range(3):
                S.dma("sp", m8[a_][pb][:], ag2_in[a_ * T + nb * 1024:a_ * T + (nb + 1) * 1024, :].rearrange("(n p) e -> p n e", p=128),
                      [], ["m8_%d_%d" % (a_, pb)])
            for i in range(8):
                n = nb * 8 + i
                q = n % 2
                S.op("dve", ["m8_0_%d" % pb, "rst", "mnwb"], ["mx_t1"],
                     lambda: V.scalar_tensor_tensor(out=t1[:], in0=m8[0][pb][:, i, :], scalar=rst[:, n:n + 1], in1=mnwb[:], op0=ALU.mult, op1=ALU.mult))
                S.op("dve", ["mx_t1", "m8_1_%d" % pb], ["mixb%d" % q], lambda: V.tensor_tensor(out=mixb[q][:, 0, :], in0=t1[:], in1=m8[1][pb][:, i, :], op=ALU.mult))
                S.op("act", ["m8_2_%d" % pb], ["mixb%d" % q], lambda: A.copy(out=mixb[q][:, 1, :], in_=m8[2][pb][:, i, :]))

                def tr2():
                    PE.transpose(out=pstb[:, 0:128], in_=mixb[q][:, 0, :], identity=identb[:])
                    return PE.transpose(out=pstb[:, 128:256], in_=mixb[q][:, 1, :], identity=identb[:])
                S.op("pe", ["mixb%d" % q, "identb"], ["pstb"], tr2)
                S.op("act", ["pstb"], ["mixT%d" % q], lambda: A.copy(out=mixT[q][:].rearrange("p a b -> p (a b)"), in_=pstb[:, 0:256]))
                for cg in range(4):
                    def mmo():
                        PE.matmul(psb[cg][:, :], lhsT=mixT[q][:, 0, :], rhs=wo[:, 0, cg * 512:(cg + 1) * 512], start=True, stop=False)
                        return PE.matmul(psb[cg][:, :], lhsT=mixT[q][:, 1, :], rhs=wo[:, 1, cg * 512:(cg + 1) * 512], start=False, stop=True)
                    S.op("pe", ["mixT%d" % q, "wo"], ["ps%d" % cg], mmo)
                    if cg % 2 == 0:
                        S.op("act", ["ps%d" % cg], ["yst%d" % q], lambda: A.copy(out=yst[q][:, cg * 512:(cg + 1) * 512], in_=psb[cg][:, :]))
                    else:
                        S.op("dve", ["ps%d" % cg], ["yst%d" % q], lambda: V.tensor_copy(out=yst[q][:, cg * 512:(cg + 1) * 512], in_=psb[cg][:, :]))
                S.dma("sp", rs1_in[n * 128:(n + 1) * 128, :], yst[q][:], ["yst%d" % q], ["rs1_in"])
        S.coll("ReduceScatter", ALU.add, rs1_in[:, :], rs1_out[:, :], ["rs1_in"], ["rs1_out"])
        S.barrier()
    es3.close()

    h1_scr = dscr("h1_scr", [TS, D])
    es6 = contextlib.ExitStack()
    bc = {}
    for nm, src in (("g1b", mod_flat[4096:6144]), ("ln1w", ln1w_in), ("ln1b", ln1b_in)):
        bc[nm] = sb("bc_" + nm, [128, D], stack=es6)
        S.dma("sp", bc[nm][:], src.partition_broadcast(128), [], ["bc_" + nm])
    S.op("dve", ["bc_g1b"], ["bc_g1b"], lambda: V.tensor_scalar_add(out=bc["g1b"][:], in0=bc["g1b"][:], scalar1=1.0))
    h1 = sb("h1", [128, NTL, D], stack=es6)
    ag3_in, ag3_out = dscr("ag3_in", [D, TS], BF16), dscr("ag3_out", [NCORES * D, TS], BF16)
    agg_in, agg_out = dscr("agg_in", [64, TS]), dscr("agg_out", [NCORES * 64, TS])
    with contextlib.ExitStack() as st:
        rwt = sb("rwt", [128, 16, 64], stack=st)
        rbb = sb("rbb", [128, 64], stack=st)
        S.dma("sp", rwt[:], rw_in.rearrange("(k p) e -> p k e", p=128), [], ["rwt"])
        S.dma("sp", rbb[:], rb_in.partition_broadcast(128), [], ["rbb"])
        u2all = sb("u2all", [128, 16, TS], BF16, stack=st)
        gTall = sb("gTall", [128, TS], stack=st)
        yt = sb("s6_yt", [128, D], stack=st)
        xt6 = sb("s6_xt", [128, D], stack=st)
        hn = sb("s6_hn", [128, D], stack=st)
        u2f = sb("s6_u2f", [128, 16, 128], stack=st)
        ln6 = layer_norm_tile(st, None, None, None, None, "b")
        sg = sb("r_s", [128, 64], stack=st)
        bi = sb("r_bi", [128, 64], stack=st)
        top8 = sb("r_top8", [128, 8, 8], stack=st)
        gsc = sb("r_gsc", [128, 8], stack=st)
        gtop = sb("r_gtop", [128, 8], stack=st)
        gmask = sb("r_gmask", [128, 8], stack=st)
        gneg = sb("r_gneg", [128, 8], stack=st)
        msk6 = sb("r_msk", [128, 64], stack=st)
        t8 = sb("r_t8", [128, 8], stack=st)
        selm = sb("r_sel", [128, 64], stack=st)
        ssum = sb("r_ssum", [128, 1], stack=st)
        gfull = sb("r_gfull", [128, 128], stack=st)
        S.op("dve", [], ["gfull"], lambda: V.memset(gfull[:], 0.0))
        R_ = "rtr"
        for i in range(NTL):
            S.dma("sp", yt[:], rs1_out[i * 128:(i + 1) * 128, :], [], ["s6_yt"])
            S.dma("sp", xt6[:], x_in[i * 128:(i + 1) * 128, :], [], ["s6_xt"])
            S.op("dve", ["s6_yt", "bc_g1b"], ["s6_yt"], lambda: V.tensor_tensor(out=yt[:], in0=yt[:], in1=bc["g1b"][:], op=ALU.mult))
            S.op("dve", ["s6_yt", "s6_xt"], ["s6_yt"],
                 lambda: V.scalar_tensor_tensor(out=yt[:], in0=xt6[:], scalar=ALPHA, in1=yt[:], op0=ALU.mult, op1=ALU.add))
            ln6(yt[:], hn[:], "s6_yt", "s6_hn")
            S.op("dve", ["s6_hn", "bc_ln1w"], ["s6_hn"], lambda: V.tensor_tensor(out=hn[:], in0=hn[:], in1=bc["ln1w"][:], op=ALU.mult))
            S.op("dve", ["s6_hn", "bc_ln1b"], ["h1_%d" % i], lambda: V.tensor_tensor(out=h1[:, i, :], in0=hn[:], in1=bc["ln1b"][:], op=ALU.add))
            ln6(h1[:, i, :], hn[:], "h1_%d" % i, "s6_hn")
            for qd in range(4):
                bank = psb[qd % 2]

                def tr4():
                    for j in range(4):
                        jj = qd * 4 + j
                        ins = PE.transpose(out=bank[:, j * 128:(j + 1) * 128], in_=hn[:, jj * 128:(jj + 1) * 128], identity=ident[:])
                    return ins
                S.op("pe", ["s6_hn", "ident"], ["ps%d" % (qd % 2)], tr4)
                for j in range(4):
                    jj = qd * 4 + j
                    S.op("act", ["ps%d" % (qd % 2), "modT"], ["s6_u2f"],
                         lambda: A.activation(out=u2f[:, jj, :], in_=bank[:, j * 128:(j + 1) * 128], func=AF.Identity,
                                              scale=modT[:, 64 + jj:65 + jj], bias=modT[:, 48 + jj:49 + jj]))
            S.op("dve", ["s6_u2f"], ["u2all"], lambda: V.tensor_copy(out=u2all[:, :, i * 128:(i + 1) * 128], in_=u2f[:]))

            def mmr():
                for k in range(16):
                    ins = PE.matmul(psb[2][:, 0:64], lhsT=u2f[:, k, :], rhs=rwt[:, k, :], start=(k == 0), stop=(k == 15))
                return ins
            S.op("pe", ["s6_u2f", "rwt"], ["ps2"], mmr)
            S.op("act", ["ps2"], [R_], lambda: A.activation(out=sg[:], in_=psb[2][:, 0:64], func=AF.Sigmoid))
            dv = lambda fn, extra=(): S.op("dve", [R_] + list(extra), [R_], fn)
            dv(lambda: V.tensor_tensor(out=bi[:], in0=sg[:], in1=rbb[:], op=ALU.add), ["rbb"])
            for g_ in range(8):
                dv(lambda: V.max(out=top8[:, g_, :], in_=bi[:, g_ * 8:(g_ + 1) * 8]))
            dv(lambda: V.tensor_tensor(out=gsc[:], in0=top8[:, :, 0], in1=top8[:, :, 1], op=ALU.add))
            dv(lambda: V.max(out=gtop[:], in_=gsc[:]))
            dv(lambda: V.tensor_scalar(out=gmask[:], in0=gsc[:], scalar1=gtop[:, 3:4], scalar2=None, op0=ALU.is_ge))
            dv(lambda: V.tensor_scalar(out=gneg[:], in0=gmask[:], scalar1=-1.0, scalar2=BIG, op0=ALU.add, op1=ALU.mult))
            for g_ in range(8):
                dv(lambda: V.tensor_scalar(out=msk6[:, g_ * 8:(g_ + 1) * 8], in0=bi[:, g_ * 8:(g_ + 1) * 8], scalar1=gmask[:, g_:g_ + 1],
                                           scalar2=gneg[:, g_:g_ + 1], op0=ALU.mult, op1=ALU.add))
            dv(lambda: V.max(out=t8[:], in_=msk6[:]))
            dv(lambda: V.tensor_scalar(out=selm[:], in0=msk6[:], scalar1=t8[:, 5:6], scalar2=None, op0=ALU.is_ge))
            dv(lambda: V.tensor_tensor(out=selm[:], in0=selm[:], in1=sg[:], op=ALU.mult))
            dv(lambda: V.tensor_reduce(out=ssum[:], in_=selm[:], axis=AX.X, op=ALU.add))
            dv(lambda: V.reciprocal(out=ssum[:], in_=ssum[:]))
            dv(lambda: V.tensor_scalar(out=gfull[:, 0:64], in0=selm[:], scalar1=ssum[:, 0:1], scalar2=2.5, op0=ALU.mult, op1=ALU.mult), ["gfull"])
            S.op("pe", [R_, "ident"], ["ps3"], lambda: PE.transpose(out=psb[3][:, 0:128], in_=gfull[:], identity=ident[:]))
            S.op("act", ["ps3"], ["gTall"], lambda: A.copy(out=gTall[:, i * 128:(i + 1) * 128], in_=psb[3][:, 0:128]))
        S.dma("sp", ag3_in.rearrange("(j p) t -> p j t", p=128), u2all[:], ["u2all"], ["ag3_in"])
        S.coll("AllGather", ALU.bypass, ag3_in[:, :], ag3_out[:, :], ["ag3_in"], ["ag3_out"])
        S.dma("sp", agg_in[:, :], gTall[0:64, :], ["gTall"], ["agg_in"])
        S.coll("AllGather", ALU.bypass, agg_in[:, :], agg_out[:, :], ["agg_in"], ["agg_out"])
        S.dma("sp", h1_scr.rearrange("(i p) d -> p i d", p=128), h1[:], ["h1_%d" % i for i in range(NTL)], ["h1_scr"])
        S.barrier()
    if stage == 6:
        dh = dout("dbg_h1", [TS, D])
        dgt = dout("dbg_gT", [64, TS])
        S.dma("sp", dh[:, :], h1_scr[:, :], [], ["dh"])
        S.dma("sp", dgt[:, :], agg_out[3 * 64:4 * 64, :], [], ["dgt"])
        S.barrier()
        S.close(); es.close()
        return nc
    es6.close()

    NKC = E_LOC * 11 + 2
    Hs = dscr("Hs", [NKC * 128, T], BF16)
    WdS = dscr("WdS", [16, 128, NKC, 128], BF16)
    rs2_in = rs1_in.rearrange("t (h c) -> (t h) c", h=2)
    rs2_out = dscr("rs2_out", [D, TS])
    with contextlib.ExitStack() as st:
        HW = 768
        WgH = [sb("WgH%d" % i, [128, 16, HW], BF16, stack=st) for i in range(2)]
        WuH = [sb("WuH%d" % i, [128, 16, HW], BF16, stack=st) for i in range(2)]
        U2 = [sb("U2_%d" % i, [128, 16, 512], BF16, stack=st) for i in range(2)]
        gblk = [sb("gblk%d" % i, [128, 512], stack=st) for i in range(2)]
        gbc = sb("gbc", [128, 512], stack=st)
        sgb = [sb("sgb%d" % i, [128, 512], stack=st) for i in range(2)]
        tbf = [sb("tbf%d" % i, [128, 512], stack=st) for i in range(2)]
        hst = [sb("hst%d" % i, [128, 6, 512], BF16, stack=st) for i in range(2)]
        esel = sb("esel_t", [128, 9, 128], stack=st)
        S.dma("sp", esel[:], esel_in.rearrange("e p m -> p e m"), [], ["esel"])
        for i in range(2):
            S.op("dve", [], ["gblk%d" % i], lambda: V.memset(gblk[i][:], 0.0))
            S.op("dve", ["gblk%d" % i], ["gblk%d" % i], lambda: V.memset(gblk[i][64:65, :], 1.0))
        jobs = []
        for e in range(E_LOC + 1):
            if e < E_LOC:
                jobs += [(e, 0, 6), (e, 6, 11)]
            else:
                jobs += [(e, 0, 2)]

        def load_w(j):
            e, h0, h1 = jobs[j]
            wbuf = j % 2
            gsrc, usrc = (eg_in[e], eu_in[e]) if e < E_LOC else (sg_in, su_in)
            for k4 in range(4):
                S.dma("pool", WgH[wbuf][:, k4 * 4:(k4 + 1) * 4, 0:(h1 - h0) * 128],
                      gsrc[k4 * 512:(k4 + 1) * 512, h0 * 128:h1 * 128].rearrange("(k p) h -> p k h", p=128), [], ["WgH%d" % wbuf])
                S.dma("pool", WuH[wbuf][:, k4 * 4:(k4 + 1) * 4, 0:(h1 - h0) * 128],
                      usrc[k4 * 512:(k4 + 1) * 512, h0 * 128:h1 * 128].rearrange("(k p) h -> p k h", p=128), [], ["WuH%d" % wbuf])
        items = [(j, tb) for j in range(len(jobs)) for tb in range(T // 512)]

        def load_u(i):
            j, tb = items[i]
            r, off = tb // 2, (tb % 2) * 512
            ub = i % 2
            S.dma("act", U2[ub][:], ag3_out[r * D:(r + 1) * D, off:off + 512].rearrange("(j p) t -> p j t", p=128), [], ["U2_%d" % ub])
            S.dma("act", gblk[ub][0:64, :], agg_out[r * 64:(r + 1) * 64, off:off + 512], [], ["gblk%d" % ub])
        wst = [sb("wst%d" % i_, [128, 2, D], BF16, stack=st) for i_ in range(2)]
        units = []
        for e_ in range(E_LOC + 1):
            nh_ = 11 if e_ < E_LOC else 2
            for h0_ in range(0, nh_, 2):
                units.append((e_, h0_, min(nh_, h0_ + 2)))

        def p0_load(k_):
            e_, h0_, h1_ = units[k_]
            b_ = k_ % 2
            src_ = ed_in[e_] if e_ < E_LOC else sd_in
            S.dma("pool", wst[b_][:, 0:h1_ - h0_, :], src_[h0_ * 128:h1_ * 128, :].rearrange("(hc p) c -> p hc c", p=128), [], ["wst%d" % b_])

        def p0_store(k_):
            e_, h0_, h1_ = units[k_]
            b_ = k_ % 2
            for hc_ in range(h0_, h1_):
                S.dma("sp", WdS[:, :, e_ * 11 + hc_, :].rearrange("cc p c -> p cc c"),
                      wst[b_][:, hc_ - h0_, :].rearrange("p (cc c) -> p cc c", c=128), ["wst%d" % b_], ["WdS"])

        def p0_unit(k_):
            if k_ > 0:
                p0_store(k_ - 1)
            if k_ < len(units):
                p0_load(k_)
        next_unit = 0
        load_w(0)
        load_u(0)
        for i, (j, tb) in enumerate(items):
            e, h0, h1 = jobs[j]
            if i % 5 == 1 and next_unit <= len(units):
                p0_unit(next_unit)
                next_unit += 1
            wbuf = j % 2
            ub = i % 2
            if tb == 0 and j + 1 < len(jobs):
                load_w(j + 1)
            if i + 1 < len(items):
                load_u(i + 1)
            S.op("pe", ["gblk%d" % ub, "esel"], ["ps6"], lambda: PE.matmul(psb[6][:, :], lhsT=esel[:, e, :], rhs=gblk[ub][:], start=True, stop=True))
            S.op("act", ["ps6"], ["gbc"], lambda: A.copy(out=gbc[:], in_=psb[6][:, :]))
            hb = i % 2
            for hc in range(h0, h1):
                pq = hc % 2
                bg, bu = psb[2 * pq], psb[2 * pq + 1]
                wc = (hc - h0) * 128

                def mmg():
                    for k in range(16):
                        ins = PE.matmul(bg[:, :], lhsT=WgH[wbuf][:, k, wc:wc + 128], rhs=U2[ub][:, k, :], start=(k == 0), stop=(k == 15))
                    return ins

                def mmu():
                    for k in range(16):
                        ins = PE.matmul(bu[:, :], lhsT=WuH[wbuf][:, k, wc:wc + 128], rhs=U2[ub][:, k, :], start=(k == 0), stop=(k == 15))
                    return ins
                S.op("pe", ["WgH%d" % wbuf, "U2_%d" % ub], ["ps%d" % (2 * pq)], mmg)
                S.op("pe", ["WuH%d" % wbuf, "U2_%d" % ub], ["ps%d" % (2 * pq + 1)], mmu)
                S.op("act", ["ps%d" % (2 * pq)], ["sgb%d" % pq], lambda: A.activation(out=sgb[pq][:], in_=bg[:, :], func=AF.Silu))
                S.op("dve", ["sgb%d" % pq, "ps%d" % (2 * pq + 1)], ["tbf%d" % pq], lambda: V.tensor_tensor(out=tbf[pq][:], in0=sgb[pq][:], in1=bu[:, :], op=ALU.mult))
                S.op("dve", ["tbf%d" % pq, "gbc"], ["hst%d" % hb], lambda: V.tensor_tensor(out=hst[hb][:, hc - h0, :], in0=tbf[pq][:], in1=gbc[:], op=ALU.mult))
            S.dma("sp", Hs[(e * 11 + h0) * 128:(e * 11 + h1) * 128, tb * 512:(tb + 1) * 512].rearrange("(hc p) t -> p hc t", p=128),
                  hst[hb][:, 0:h1 - h0, :], ["hst%d" % hb], ["Hs"])
        while next_unit <= len(units):
            p0_unit(next_unit)
            next_unit += 1
        S.barrier()
    with contextlib.ExitStack() as st:
        Hblk = sb("Hblk", [128, NKC, 512], BF16, stack=st)
        Wd = [sb("Wd%d" % i, [128, NKC, 128], BF16, stack=st) for i in range(2)]
        ys2 = [sb("ys2_%d" % i, [128, 512], stack=st) for i in range(2)]
        cnt = 0
        for tb in range(T // 512):
            r, off = tb // 2, (tb % 2) * 512
            for k0 in range(0, NKC, 30):
                k1 = min(NKC, k0 + 30)
                S.dma("sp", Hblk[:, k0:k1, :],
                      Hs[k0 * 128:k1 * 128, tb * 512:(tb + 1) * 512].rearrange("(kc p) t -> p kc t", p=128), [], ["Hblk"])
            for cc in range(16):
                wb = cnt % 2
                if cnt == 0:
                    S.dma("act", Wd[0][:], WdS[0, :, :, :], [], ["Wd0"])
                cnt += 1
                if not (tb == T // 512 - 1 and cc == 15):
                    S.dma("act", Wd[1 - wb][:], WdS[(cc + 1) % 16, :, :, :], [], ["Wd%d" % (1 - wb)])
                bank = psb[wb]

                def mmd():
                    for kc in range(NKC):
                        ins = PE.matmul(bank[:, :], lhsT=Wd[wb][:, kc, :], rhs=Hblk[:, kc, :], start=(kc == 0), stop=(kc == NKC - 1))
                    return ins
                S.op("pe", ["Wd%d" % wb, "Hblk"], ["ps%d" % wb], mmd)
                if wb == 0:
                    S.op("act", ["ps0"], ["ys2_0"], lambda: A.copy(out=ys2[0][:], in_=bank[:, :]))
                else:
                    S.op("dve", ["ps1"], ["ys2_1"], lambda: V.tensor_copy(out=ys2[1][:], in_=bank[:, :]))
                S.dma("sp", rs2_in[r * D + cc * 128:r * D + (cc + 1) * 128, off:off + 512], ys2[wb][:], ["ys2_%d" % wb], ["rs2_in"])
        S.coll("ReduceScatter", ALU.add, rs2_in, rs2_out[:, :], ["rs2_in"], ["rs2_out"])
        S.barrier()
    if os.environ.get("MK_DBG"):
        dy = dout("dbg_yT", [D, TS])
        S.dma("sp", dy[:, :], rs2_out[:, :], [], ["dy"])
        dh = dout("dbg_Hs", [NKC * 128, 1024], BF16)
        S.dma("sp", dh[:, :], Hs[:, 2048:3072], [], ["dh"])
        S.barrier()

    with contextlib.ExitStack() as st:
        bc = {}
        for nm, src in (("g2b", mod_flat[10240:12288]), ("ln2w", ln2w_in), ("ln2b", ln2b_in)):
            bc[nm] = sb("bc8_" + nm, [128, D], stack=st)
            S.dma("sp", bc[nm][:], src.partition_broadcast(128), [], ["bc_" + nm])
        S.op("dve", ["bc_g2b"], ["bc_g2b"], lambda: V.tensor_scalar_add(out=bc["g2b"][:], in0=bc["g2b"][:], scalar1=1.0))
        yT8 = sb("yT8", [128, 16, 128], stack=st)
        y8 = sb("y8", [128, D], stack=st)
        h8 = sb("h8", [128, D], stack=st)
        o8 = [sb("o8_%d" % i, [128, D], stack=st) for i in range(2)]
        ln8 = layer_norm_tile(st, None, None, None, None, "c")
        for i in range(NTL):
            ob = i % 2
            S.dma("sp", yT8[:], rs2_out[:, i * 128:(i + 1) * 128].rearrange("(j p) t -> p j t", p=128), [], ["yT8"])
            S.dma("sp", h8[:], h1_scr[i * 128:(i + 1) * 128, :], [], ["h8"])
            for qd in range(4):
                def tr8():
                    for j in range(4):
                        ins = PE.transpose(out=psb[qd][:, j * 128:(j + 1) * 128], in_=yT8[:, qd * 4 + j, :], identity=ident[:])
                    return ins
                S.op("pe", ["yT8", "ident"], ["ps%d" % qd], tr8)
                S.op("dve", ["ps%d" % qd, "bc_g2b"], ["y8"],
                     lambda: V.tensor_tensor(out=y8[:, qd * 512:(qd + 1) * 512], in0=psb[qd][:, :], in1=bc["g2b"][:, qd * 512:(qd + 1) * 512], op=ALU.mult))
            S.op("dve", ["y8", "h8"], ["y8"], lambda: V.scalar_tensor_tensor(out=y8[:], in0=h8[:], scalar=ALPHA, in1=y8[:], op0=ALU.mult, op1=ALU.add))
            ln8(y8[:], o8[ob][:], "y8", "o8_%d" % ob)
            S.op("dve", ["o8_%d" % ob, "bc_ln2w"], ["o8_%d" % ob], lambda: V.tensor_tensor(out=o8[ob][:], in0=o8[ob][:], in1=bc["ln2w"][:], op=ALU.mult))
            S.op("dve", ["o8_%d" % ob, "bc_ln2b"], ["o8_%d" % ob], lambda: V.tensor_tensor(out=o8[ob][:], in0=o8[ob][:], in1=bc["ln2b"][:], op=ALU.add))
            S.dma("sp", out_ap[i * 128:(i + 1) * 128, :], o8[ob][:], ["o8_%d" % ob], ["out"])
        S.barrier()
    S.close(); es.close()
    return nc


def _bf16(a):
    return a.astype(ml_dtypes.bfloat16)


def make_in_maps(inp, stage=99):
    f = np.float32
    x = np.asarray(inp["x"], f)[0]
    w_in = np.asarray(inp["w_in"], f)[0]
    ident = np.eye(128, dtype=f)
    masks = np.zeros((6, 128, 128), f)
    jj, ii = np.meshgrid(np.arange(128), np.arange(128), indexing="ij")
    masks[0] = (jj <= ii)
    masks[1] = (jj >= ii)
    masks[2][:64, :64] = (jj < ii)[:64, :64]
    masks[3][:64, :64] = (jj > ii)[:64, :64]
    mneg = np.zeros((2, 4, 128, 512), f)
    jp, tt = np.meshgrid(np.arange(128), np.arange(512), indexing="ij")
    for r in range(4):
        mneg[0, r] = np.where(128 * r + jp <= tt, 0.0, -BIG)
        mneg[1, r] = np.where(128 * r + jp >= tt, 0.0, -BIG)
    maps = []
    for c in range(NCORES):
        h, g = c // 2, c
        cols = []
        cols += list(range(3088 + g * 128, 3088 + g * 128 + 128))
        cols += list(range(3088 + 1024 + g * 128, 3088 + 1024 + g * 128 + 128))
        cols += list(range(3088 + 2048 + g * 128, 3088 + 2048 + g * 128 + 128))
        cols += list(range(h * 128, h * 128 + 128))
        cols += list(range(512 + h * 128, 512 + h * 128 + 128))
        vh = c % 2
        cols += list(range(1024 + h * 256 + vh * 128, 1024 + h * 256 + vh * 128 + 128))
        cols += list(range(2048 + h * 256 + vh * 128, 2048 + h * 256 + vh * 128 + 128))
        cols += list(range(6160 + g * 128, 6160 + g * 128 + 128))
        cols += [3072 + h, 3072 + 4 + h, 3080 + h, 3080 + 4 + h, 7184 + g, 7184 + 8 + g, 7200 + g, 7200 + 8 + g]
        assert len(cols) == NW
        esel = np.zeros((9, 128, 128), f)
        for j in range(E_LOC):
            esel[j, c * E_LOC + j, :] = 1.0
        esel[E_LOC, 64, :] = 1.0
        gate_bias = np.array([inp["m_igate_bias"][0, 0, h], inp["m_igate_bias"][0, 1, h],
                              inp["m_fgate_bias"][0, 0, h], inp["m_fgate_bias"][0, 1, h], 0.0, 0.0,
                              inp["g_dt_bias"][0, 0, g], inp["g_dt_bias"][0, 1, g],
                              inp["g_A_log"][0, 0, g], inp["g_A_log"][0, 1, g], 0, 0, 0, 0, 0, 0], f)
        cw = np.asarray(inp["g_conv_w"], f)[0]
        conv_w = np.stack([cw[:, s * 1024 + g * 128:s * 1024 + g * 128 + 128].T for s in range(3)], 0)
        m = {
            "x": np.ascontiguousarray(x[c * TS:(c + 1) * TS]),
            "c": np.asarray(inp["c"], f)[0],
            "w_ada": np.ascontiguousarray(np.asarray(inp["w_ada"], f)[0][:, c * 1536:(c + 1) * 1536]),
            "b_ada": np.asarray(inp["b_ada"], f)[0][None, c * 1536:(c + 1) * 1536],
            "w_in": np.ascontiguousarray(w_in[:, cols]),
            "gate_bias": gate_bias,
            "m_norm_w": np.asarray(inp["m_norm_w"], f)[0][h * 256 + vh * 128:h * 256 + vh * 128 + 128].copy(),
            "sel_head": np.array([1.0 if cc // 2 == h else 0.0 for cc in range(NCORES)], f),
            "esel": esel,
            "g_norm_w": np.asarray(inp["g_norm_w"], f)[0].copy(),
            "conv_w": np.ascontiguousarray(conv_w),
            "w_out": np.ascontiguousarray(np.concatenate([np.asarray(inp["w_out"], f)[0][h * 256 + vh * 128:h * 256 + vh * 128 + 128],
                                                          np.asarray(inp["w_out"], f)[0][1024 + g * 128:1024 + g * 128 + 128]], 0)),
            "ln1_w": np.asarray(inp["ln1_w"], f)[0], "ln1_b": np.asarray(inp["ln1_b"], f)[0],
            "ln2_w": np.asarray(inp["ln2_w"], f)[0], "ln2_b": np.asarray(inp["ln2_b"], f)[0],
            "router_w": np.asarray(inp["router_w"], f)[0], "router_bias": np.asarray(inp["router_bias"], f)[0],
            "ident": ident, "masks": masks, "mneg": mneg,
        }
        if stage >= 7:
            m["e_gate"] = np.asarray(inp["e_gate"])[0, c * E_LOC:(c + 1) * E_LOC]
            m["e_up"] = np.asarray(inp["e_up"])[0, c * E_LOC:(c + 1) * E_LOC]
            m["e_down"] = np.asarray(inp["e_down"])[0, c * E_LOC:(c + 1) * E_LOC]
            def padc(a_):
                o_ = np.zeros((a_.shape[0], 256), f); o_[:, :DSH] = a_; return o_
            m["s_gate"] = padc(np.asarray(inp["s_gate"], f)[0][:, c * DSH:(c + 1) * DSH])
            m["s_up"] = padc(np.asarray(inp["s_up"], f)[0][:, c * DSH:(c + 1) * DSH])
            sdp = np.zeros((256, D), f); sdp[:DSH] = np.asarray(inp["s_down"], f)[0][c * DSH:(c + 1) * DSH, :]
            m["s_down"] = sdp
        maps.append(m)
    return maps


def run_stage(inp, stage):
    nc = build_program(stage)
    maps = make_in_maps(inp, stage)
    res = run_bass_kernel_spmd(nc, maps, core_ids=list(range(NCORES)))
    return res.results


def kernel(**inputs):
    res = run_stage(inputs, 99)
    out = np.concatenate([np.asarray(r["out"]) for r in res], axis=0)
    return out.reshape(1, T, D).astype(np.float32)
```
